# Optimizing a Trainium2 kernel written in Bass

```python
import math
import jax, jax.numpy as jnp
from jax import lax
import numpy as np

D_MODEL = 2048
BATCH = 2
SEQ = 4096
DEPTH = 1

HEAD_DIM = 64
ATTN_WIDTH = D_MODEL // 2
N_Q_HEADS = ATTN_WIDTH // HEAD_DIM
N_KV_HEADS = N_Q_HEADS // 4
KV_WIDTH = N_KV_HEADS * HEAD_DIM
CONV_WIDTH = D_MODEL - ATTN_WIDTH
CONV_K = 3
WINDOW = 128
Q_BLOCK = 128
IN_WIDTH = ATTN_WIDTH + 2 * KV_WIDTH + 3 * CONV_WIDTH
N_EXPERTS = 64
TOP_K = 8
N_GROUPS = 8
TOPK_GROUPS = 4
D_EXPERT = 512
ROUTED_SCALE = 2.5
EXPERT_BLOCK = 128
ALPHA = (2.0 * DEPTH) ** 0.25
BETA = (8.0 * DEPTH) ** -0.25
LN_EPS = 1e-5

kernel_name = "hymba_conv_swa_moe_deepnorm_adaln"


def layer_norm(x, g, b):
    xf = x.astype(jnp.float32)
    mu = jnp.mean(xf, axis=-1, keepdims=True)
    var = jnp.mean(jnp.square(xf - mu), axis=-1, keepdims=True)
    y = (xf - mu) * lax.rsqrt(var + LN_EPS) * g.astype(jnp.float32) + b.astype(jnp.float32)
    return y.astype(x.dtype)


def alibi_slopes(n_heads):
    return 2.0 ** (-8.0 * jnp.arange(1, n_heads + 1, dtype=jnp.float32) / n_heads)


def swa_sink_attention(q, k, v, sinks):
    b, s = q.shape[0], q.shape[1]
    nb = s // Q_BLOCK
    g = N_Q_HEADS // N_KV_HEADS
    qb = q.reshape(b, nb, Q_BLOCK, N_KV_HEADS, g, HEAD_DIM)

    def band(t):
        tb = t.reshape(b, nb, Q_BLOCK, N_KV_HEADS, HEAD_DIM)
        prev = jnp.pad(tb, ((0, 0), (1, 0), (0, 0), (0, 0), (0, 0)))[:, :-1]
        return jnp.concatenate([prev, tb], axis=2)

    kk, vv = band(k), band(v)
    scores = jnp.einsum('bnqhgd,bnkhd->bnhgqk', qb, kk,
                        preferred_element_type=jnp.float32) * (HEAD_DIM ** -0.5)
    qi = jnp.arange(Q_BLOCK)[:, None]
    kj = jnp.arange(2 * Q_BLOCK)[None, :]
    dist = qi + Q_BLOCK - kj
    valid = (dist >= 0) & (dist < WINDOW)
    valid = valid[None] & ((jnp.arange(nb)[:, None, None] > 0) | (kj[None] >= Q_BLOCK))
    slopes = alibi_slopes(N_Q_HEADS).reshape(N_KV_HEADS, g)
    scores = scores - slopes[:, :, None, None] * dist.astype(jnp.float32)
    scores = jnp.where(valid[None, :, None, None], scores, -jnp.inf)
    sink = sinks.astype(jnp.float32).reshape(N_KV_HEADS, g)[None, None, :, :, None, None]
    m = jnp.maximum(jnp.max(scores, axis=-1, keepdims=True), sink)
    p = jnp.exp(scores - m)
    denom = jnp.sum(p, axis=-1, keepdims=True) + jnp.exp(sink - m)
    p = (p / denom).astype(v.dtype)
    out = jnp.einsum('bnhgqk,bnkhd->bnqhgd', p, vv)
    return out.reshape(b, s, ATTN_WIDTH)


def short_gated_conv(gate_b, gate_c, hc, conv_w):
    u = gate_c * hc
    up = jnp.pad(u, ((0, 0), (CONV_K - 1, 0), (0, 0)))
    s = u.shape[1]
    y = sum(conv_w[j] * up[:, j:j + s] for j in range(CONV_K))
    return gate_b * y


def swiglu(h, wg, wu, wd):
    return (jax.nn.silu(h @ wg) * (h @ wu)) @ wd


def routed_moe(h, w_router, router_bias, w_gate, w_up, w_down):
    n, d = h.shape
    scores = jax.nn.sigmoid((h @ w_router).astype(jnp.float32))
    sel = scores + router_bias.astype(jnp.float32)
    grp = sel.reshape(n, N_GROUPS, N_EXPERTS // N_GROUPS)
    gscore = jnp.sum(lax.top_k(grp, 2)[0], axis=-1)
    _, gidx = lax.top_k(gscore, TOPK_GROUPS)
    gmask = jnp.sum(jax.nn.one_hot(gidx, N_GROUPS, dtype=jnp.int32), axis=1) > 0
    emask = jnp.repeat(gmask, N_EXPERTS // N_GROUPS, axis=1)
    _, eidx = lax.top_k(jnp.where(emask, sel, -jnp.inf), TOP_K)
    w = jnp.take_along_axis(scores, eidx, axis=1)
    w = (w / jnp.sum(w, axis=-1, keepdims=True) * ROUTED_SCALE).astype(h.dtype)

    a = n * TOP_K
    flat_e = eidx.reshape(-1).astype(jnp.int32)
    flat_w = w.reshape(-1)
    order = jnp.argsort(flat_e)
    sorted_e = flat_e[order]
    counts = jnp.bincount(flat_e, length=N_EXPERTS)
    offsets = jnp.cumsum(counts) - counts
    rank = jnp.arange(a, dtype=jnp.int32) - offsets[sorted_e]
    padded = (counts + EXPERT_BLOCK - 1) // EXPERT_BLOCK * EXPERT_BLOCK
    pad_end = jnp.cumsum(padded)
    dest = (pad_end - padded)[sorted_e] + rank
    n_rows = (a + EXPERT_BLOCK - 1) // EXPERT_BLOCK * EXPERT_BLOCK + N_EXPERTS * EXPERT_BLOCK
    n_blk = n_rows // EXPERT_BLOCK
    row_tok = jnp.zeros((n_rows,), jnp.int32).at[dest].set((order // TOP_K).astype(jnp.int32))
    row_w = jnp.zeros((n_rows,), h.dtype).at[dest].set(flat_w[order])
    block_e = jnp.minimum(jnp.searchsorted(pad_end, jnp.arange(n_blk) * EXPERT_BLOCK, side='right'),
                          N_EXPERTS - 1).astype(jnp.int32)

    def expert_block(args):
        tok, wr, e = args
        xb = h[tok]
        return swiglu(xb, w_gate[e], w_up[e], w_down[e]) * wr[:, None]

    ys = lax.map(expert_block, (row_tok.reshape(n_blk, EXPERT_BLOCK),
                                row_w.reshape(n_blk, EXPERT_BLOCK), block_e))
    return jnp.zeros_like(h).at[row_tok].add(ys.reshape(n_rows, d))


def setup_inputs(seed: int = 0) -> dict:
    key = jax.random.key(seed)
    ks = jax.random.split(key, 20)
    f32 = jnp.float32
    D, L = D_MODEL, DEPTH
    nrm = lambda k, shp, s: jax.random.normal(k, shp, f32) * s
    col_scale = jnp.concatenate([
        jnp.ones((ATTN_WIDTH + KV_WIDTH,), f32), jnp.full((KV_WIDTH,), BETA, f32),
        jnp.ones((2 * CONV_WIDTH,), f32), jnp.full((CONV_WIDTH,), BETA, f32)])
    return {
        "x": jax.random.normal(ks[0], (BATCH, SEQ, D), f32),
        "c": jax.random.normal(ks[1], (BATCH, D), f32),
        "w_mod": nrm(ks[2], (L, D, 6 * D), 0.5 * D ** -0.5),
        "b_mod": nrm(ks[3], (L, 6 * D), 0.02),
        "w_in": nrm(ks[4], (L, D, IN_WIDTH), D ** -0.5) * col_scale,
        "conv_w": nrm(ks[5], (L, CONV_K, CONV_WIDTH), CONV_K ** -0.5),
        "attn_sinks": nrm(ks[6], (L, N_Q_HEADS), 0.5),
        "w_out": nrm(ks[7], (L, D, D), BETA * D ** -0.5),
        "ln1_g": 1.0 + nrm(ks[8], (L, D), 0.02),
        "ln1_b": nrm(ks[9], (L, D), 0.02),
        "w_router": nrm(ks[10], (L, D, N_EXPERTS), D ** -0.5),
        "router_bias": nrm(ks[11], (L, N_EXPERTS), 0.01),
        "w_gate": nrm(ks[12], (L, N_EXPERTS, D, D_EXPERT), BETA * D ** -0.5),
        "w_up": nrm(ks[13], (L, N_EXPERTS, D, D_EXPERT), BETA * D ** -0.5),
        "w_down": nrm(ks[14], (L, N_EXPERTS, D_EXPERT, D), BETA * D_EXPERT ** -0.5),
        "ws_gate": nrm(ks[15], (L, D, D_EXPERT), BETA * D ** -0.5),
        "ws_up": nrm(ks[16], (L, D, D_EXPERT), BETA * D ** -0.5),
        "ws_down": nrm(ks[17], (L, D_EXPERT, D), BETA * D_EXPERT ** -0.5),
        "ln2_g": 1.0 + nrm(ks[18], (L, D), 0.02),
        "ln2_b": nrm(ks[19], (L, D), 0.02),
    }


def reference(x, c, w_mod, b_mod, w_in, conv_w, attn_sinks, w_out, ln1_g, ln1_b,
              w_router, router_bias, w_gate, w_up, w_down, ws_gate, ws_up, ws_down,
              ln2_g, ln2_b):
    b, s, d = x.shape
    cs = jax.nn.silu(c)
    splits = [ATTN_WIDTH, ATTN_WIDTH + KV_WIDTH, ATTN_WIDTH + 2 * KV_WIDTH,
              ATTN_WIDTH + 2 * KV_WIDTH + CONV_WIDTH, ATTN_WIDTH + 2 * KV_WIDTH + 2 * CONV_WIDTH]
    for l in range(DEPTH):
        mod = (cs @ w_mod[l] + b_mod[l])[:, None, :]
        sh1, sc1, g1, sh2, sc2, g2 = jnp.split(mod, 6, axis=-1)
        h = x * (1.0 + sc1) + sh1
        proj = h @ w_in[l]
        q, k, v, cb, cc, ch = jnp.split(proj, splits, axis=-1)
        attn = swa_sink_attention(q.reshape(b, s, N_Q_HEADS, HEAD_DIM),
                                  k.reshape(b, s, N_KV_HEADS, HEAD_DIM),
                                  v.reshape(b, s, N_KV_HEADS, HEAD_DIM), attn_sinks[l])
        conv = short_gated_conv(cb, cc, ch, conv_w[l])
        mix = jnp.concatenate([attn, conv], axis=-1) @ w_out[l]
        x = layer_norm(ALPHA * x + (1.0 + g1) * mix, ln1_g[l], ln1_b[l])
        h = (x * (1.0 + sc2) + sh2).reshape(b * s, d)
        ffn = routed_moe(h, w_router[l], router_bias[l], w_gate[l], w_up[l], w_down[l]) \
            + swiglu(h, ws_gate[l], ws_up[l], ws_down[l])
        x = layer_norm(ALPHA * x + (1.0 + g2) * ffn.reshape(b, s, d), ln2_g[l], ln2_b[l])
    return x
```

```python
import contextlib
import numpy as np
import concourse.bass as bass
import concourse.mybir as mybir
from concourse.bass_utils import run_bass_kernel_spmd

F32 = mybir.dt.float32
BF16 = mybir.dt.bfloat16
AF = mybir.ActivationFunctionType
ALU = mybir.AluOpType
AX = mybir.AxisListType

NCORES = 8
D = 2048
KC = 16
TOWN = 1024
TALL = 1152
NT = 8
INW = 4608
NE = 64
DEXP = 512
ALPHA = 2.0 ** 0.25
LN_EPS = 1e-5
ROUTED_SCALE = 2.5
NEG = -30000.0
CAP = 512
NS = CAP // 128
BIG = 1.0e6
I32 = mybir.dt.int32
U32 = mybir.dt.uint32


class Sem:
    def __init__(self, h):
        self.h = h
        self.n = 0


class Prog:
    ENG = ("tensor", "vector", "scalar", "gpsimd", "sync")

    def __init__(self, nc, es, tag):
        self.nc = nc
        self.es = es
        self.tag = tag
        self.q = {e: [] for e in self.ENG}
        self.last = {e: None for e in self.ENG}
        self.nsem = 0
        self.handles = []
        self.selfsem = {e: self.sem("self_" + e) for e in ("vector", "scalar", "gpsimd")}

    def sem(self, name=None):
        self.nsem += 1
        h = self.nc.alloc_semaphore(name=f"{self.tag}_{name or 's'}_{self.nsem}")
        self.handles.append(h)
        return Sem(h)

    def op(self, eng, fn, sem=None, dma=False):
        if sem is None and not dma and eng in self.selfsem:
            sem = self.selfsem[eng]
        if sem is None:
            self.q[eng].append(("op", fn, None, 0))
            return None
        inc = 16 if dma else 1
        sem.n += inc
        self.q[eng].append(("op", fn, sem, inc))
        if not dma:
            self.last[eng] = (sem, sem.n)
        return sem.n

    def raw(self, eng, fn):
        self.q[eng].append(("raw", fn))

    def semring(self, name, n):
        return [self.sem(f"{name}{i}") for i in range(n)]

    def wait(self, eng, sem, val=None):
        if val is None:
            val = sem.n
        assert val <= sem.n, (eng, val, sem.n)
        if val <= 0:
            return
        self.q[eng].append(("wait", sem, val))

    def seq(self, eng):
        if self.last[eng] is not None:
            self.wait(eng, *self.last[eng])

    def emit(self):
        with self.nc.Block() as block:
            for eng in self.ENG:
                items = self.q[eng]
                if not items:
                    continue

                def body(e, items=items):
                    for it in items:
                        if it[0] == "wait":
                            e.wait_ge(it[1].h, it[2])
                        elif it[0] == "raw":
                            it[1](e)
                        else:
                            ins = it[1](e)
                            if it[2] is not None:
                                ins.then_inc(it[2].h, it[3])

                getattr(block, eng)(body)
        self.nc.clear_and_free_semaphores(self.handles)
        self.nc.all_engine_barrier()

    def mm(self, out, lhsT, rhs, start, stop, sem=None):
        return self.op("tensor", lambda e: e.matmul(out, lhsT=lhsT, rhs=rhs, start=start, stop=stop), sem)

    def tr(self, out, in_, ident, sem=None):
        return self.op("tensor", lambda e: e.transpose(out=out, in_=in_, identity=ident), sem)

    def act(self, out, in_, func, sem=None, **kw):
        return self.op("scalar", lambda e: e.activation(out=out, in_=in_, func=func, **kw), sem)

    def tt(self, out, in0, in1, op, sem=None, eng="vector"):
        return self.op(eng, lambda e: e.tensor_tensor(out=out, in0=in0, in1=in1, op=op), sem)

    def ts(self, out, in0, s1, s2, op0, op1=None, sem=None, eng="vector"):
        if op1 is None:
            return self.op(eng, lambda e: e.tensor_scalar(out=out, in0=in0, scalar1=s1, scalar2=None, op0=op0), sem)
        return self.op(eng, lambda e: e.tensor_scalar(out=out, in0=in0, scalar1=s1, scalar2=s2, op0=op0, op1=op1), sem)

    def stt(self, out, in0, scalar, in1, op0, op1, sem=None):
        return self.op("vector", lambda e: e.scalar_tensor_tensor(out=out, in0=in0, scalar=scalar, in1=in1, op0=op0, op1=op1), sem)

    def copy(self, eng, out, in_, sem=None):
        if eng == "scalar":
            return self.op("scalar", lambda e: e.copy(out=out, in_=in_), sem)
        return self.op(eng, lambda e: e.tensor_copy(out=out, in_=in_), sem)

    def dma(self, eng, out, in_, sem):
        return self.op(eng, lambda e: e.dma_start(out=out, in_=in_), sem, dma=True)


class BankRing:
    def __init__(self, p, banks):
        self.p = p
        self.banks = list(banks)
        self.i = 0
        self.cond = {b: [] for b in self.banks}

    def take(self):
        b = self.banks[self.i % len(self.banks)]
        self.i += 1
        for (s, v) in self.cond[b]:
            self.p.wait("tensor", s, v)
        self.cond[b] = []
        return b

    def release(self, b, sem, val):
        self.cond[b].append((sem, val))


_DECLARED = []


def build_program(debug=False, upto=99):
    nc = bass.Bass("TRN2", target_bir_lowering=False)
    es = contextlib.ExitStack()

    _DECLARED.clear()

    def din(name, shape, dt=F32):
        if (upto < 7 and name in ("ws_gate", "ws_up", "ws_down")) or (upto < 8 and name in ("w_gate", "w_up", "w_down")):
            return None
        _DECLARED.append(name)
        return nc.dram_tensor(name, list(shape), dt, kind="ExternalInput").ap()

    xh = din("xh", [TALL, D])
    ccol = din("ccol", [128, KC])
    w_mod = din("w_mod", [D, 6 * D])
    b_mod = din("b_mod", [1, 6 * D])
    bmod_col = din("bmod_col", [128, 96])
    w_in = din("w_in", [D, INW])
    convw = din("convw", [128, 8, 3])
    sinks = din("sinks", [128, 8])
    w_out = din("w_out", [D, D])
    ln1_g = din("ln1_g", [1, D])
    ln1_b = din("ln1_b", [1, D])
    ln2_g = din("ln2_g", [1, D])
    ln2_b = din("ln2_b", [1, D])
    w_router = din("w_router", [D, NE])
    router_bias = din("router_bias", [1, NE])
    w_gate = din("w_gate", [NE, D, DEXP])
    w_up = din("w_up", [NE, D, DEXP])
    w_down = din("w_down", [NE, DEXP, D])
    ws_gate = din("ws_gate", [D, DEXP])
    ws_up = din("ws_up", [D, DEXP])
    ws_down = din("ws_down", [DEXP, D])
    abias = din("abias", [128, 4, 2, 512])
    flags = din("flags", [128, 2])
    ident_d = din("ident_in", [128, 128])
    y = nc.dram_tensor("y", [TOWN, D], F32, kind="ExternalOutput").ap()
    x1s = nc.dram_tensor("x1s", [TOWN, D], F32, kind="Internal").ap()
    ysh = nc.dram_tensor("ysh", [TOWN, D], F32, kind="Internal").ap()
    hd = nc.dram_tensor("hd", [NE * CAP, D], BF16, kind="Internal").ap()
    yd = nc.dram_tensor("yd", [NE * CAP, D], F32, kind="Internal").ap()
    ltri_d = din("ltri", [128, 128])
    ebase_d = din("ebase", [128, NE])
    pcol_d = din("pcol", [128, NS])
    dbg = {}
    if debug:
        dbg["d_hT"] = nc.dram_tensor("d_hT", [128, KC, TALL], BF16, kind="ExternalOutput").ap()
        dbg["d_cat"] = nc.dram_tensor("d_cat", [128, KC, TOWN], BF16, kind="ExternalOutput").ap()
        dbg["d_x1"] = nc.dram_tensor("d_x1", [TOWN, D], F32, kind="ExternalOutput").ap()
        dbg["d_G"] = nc.dram_tensor("d_G", [128, NT, NE + 1], F32, kind="ExternalOutput").ap()
        dbg["d_colv"] = nc.dram_tensor("d_colv", [128, 64], F32, kind="ExternalOutput").ap()
        dbg["d_g1p"] = nc.dram_tensor("d_g1p", [128, D], F32, kind="ExternalOutput").ap()
        dbg["d_dest"] = nc.dram_tensor("d_dest", [128, NT, 8], I32, kind="ExternalOutput").ap()
        dbg["d_wk"] = nc.dram_tensor("d_wk", [128, NT, 8], F32, kind="ExternalOutput").ap()

    _names = {}

    def sb(name, shape, dt, side="left"):
        _names[name] = _names.get(name, 0) + 1
        if _names[name] > 1:
            name = f"{name}_{_names[name]}"
        g = nc.sbuf_tensor(name, list(shape), dt, side=side)
        t = g.__enter__()
        return g, t

    def free(g):
        g.__exit__(None, None, None)

    _, ident = sb("ident", [128, 128], F32)
    _, colv = sb("colv", [128, 64], F32)
    _, g1p = sb("g1p", [128, D], F32)
    _, g2p = sb("g2p", [128, D], F32)
    _, flg = sb("flg", [128, 2], F32)
    _, onesb = sb("onesb", [128, 64], BF16)
    _, G = sb("G", [128, NT, NE + 1], F32)
    _, sh2b = sb("sh2b", [128, D], F32)
    _, sc2b = sb("sc2b", [128, D], F32)
    _, dest_i = sb("dest_i", [128, NT, 8], I32)
    _, wk = sb("wk", [128, NT, 8], F32)
    _, idx_i = sb("idx_i", [128, NS, NE], I32)
    _, cs_bf = sb("cs_bf", [128, KC], BF16)
    _, ones_row = sb("ones_row", [1, 128], F32)
    _, bmc = sb("bmc", [128, 96], F32)
    ps_g = nc.psum_tensor("ps", [128, 8, 512], F32)
    ps = ps_g.__enter__()

    def bank(i):
        return ps[:, i, :]

    SH1, SC1, SH2, SC2 = 0, 16, 32, 48

    p = Prog(nc, es, "p0")
    g_cc, cc_t = sb("cc_t", [128, KC], F32)
    g_wm, wm = sb("wm", [128, 2, KC, 512], BF16)
    s_ld = p.sem("ld")
    s_w = p.semring("w", 2)
    s_mm = p.sem("mm")
    s_act = p.sem("act")
    p.dma("sync", cc_t[:], ccol[:, :], s_ld)
    p.dma("sync", bmc[:], bmod_col[:, :], s_ld)
    p.dma("sync", ident[:], ident_d[:, :], s_ld)
    p.dma("sync", flg[:], flags[:, :], s_ld)
    p.wait("scalar", s_ld)
    v_cs = p.act(cs_bf[:], cc_t[:], AF.Silu, sem=s_act)
    p.op("vector", lambda e: e.memset(ones_row[:], 1.0))
    p.op("vector", lambda e: e.memset(onesb[:], 1.0))
    p.op("vector", lambda e: e.memset(G[:], 1.0))
    wmv = w_mod.rearrange("(kc p) n -> p kc n", p=128)
    NB0 = 8
    wfull = {}
    wfree = {}

    def issue_wm(b):
        if b >= 2:
            p.wait("gpsimd", s_mm, wfree[b - 2])
        wfull[b] = p.dma("gpsimd", wm[:, b % 2], wmv[:, :, b * 512:(b + 1) * 512], s_w[b % 2])

    issue_wm(0)
    issue_wm(1)
    p.wait("tensor", s_act, v_cs)
    p.wait("vector", s_ld)
    for b in range(NB0):
        p.wait("tensor", s_w[b % 2], wfull[b])
        seg = b // 4
        for c4 in range(4):
            idx = seg * 16 + (b % 4) * 4 + c4
            for kc in range(KC):
                v = p.mm(ps[:, 2, idx:idx + 1], wm[:, b % 2, kc, c4 * 128:(c4 + 1) * 128], cs_bf[:, kc:kc + 1],
                         kc == 0, kc == KC - 1, sem=s_mm if (kc == KC - 1 and c4 == 3) else None)
        wfree[b] = v
        if b + 2 < NB0:
            issue_wm(b + 2)
    p.wait("vector", s_mm, v)
    for gi, c0 in enumerate((0, 16)):
        p.tt(colv[:, gi * 16:(gi + 1) * 16], ps[:, 2, gi * 16:(gi + 1) * 16], bmc[:, c0:c0 + 16], ALU.add)
    p.seq("vector")
    p.ts(colv[:, 16:32], colv[:, 16:32], 1.0, None, ALU.add)
    p.emit()
    for g in (g_wm, g_cc):
        free(g)
    if upto == 0:
        return nc

    g_cat, catT = sb("catT", [128, KC, TOWN], BF16, side="right")
    g_q, qT = sb("qT", [128, 8, TOWN], BF16)
    g_k, kT = sb("kT", [128, 4, TALL], BF16)
    g_v, V = sb("V", [128, 9, 256], BF16)
    g_h, hT = sb("hT", [128, KC, TALL], BF16)

    p = Prog(nc, es, "p1")
    g_xs, xs = sb("xs", [128, 2, D], F32)
    s_x = p.semring("x", 2)
    s_t = p.sem("t")
    s_ea = p.sem("ea")
    s_ev = p.sem("ev")
    s_st = p.sem("st")
    xfull = {}
    tdone = {}

    def issue_x(tt_):
        if tt_ >= 2:
            p.wait("sync", s_t, tdone[tt_ - 2])
        xfull[tt_] = p.dma("sync", xs[:, tt_ % 2, :], xh[tt_ * 128:(tt_ + 1) * 128, :], s_x[tt_ % 2])

    issue_x(0)
    issue_x(1)
    ea_val = {}
    ev_val = {}
    for tt_ in range(9):
        p.wait("tensor", s_x[tt_ % 2], xfull[tt_])
        if tt_ >= 2:
            p.wait("tensor", s_ea, ea_val[tt_ - 2])
            p.wait("tensor", s_ev, ev_val[tt_ - 2])
        for kc in range(KC):
            bk = (tt_ % 2) * 4 + kc // 4
            v = p.tr(ps[:, bk, (kc % 4) * 128:(kc % 4 + 1) * 128], xs[:, tt_ % 2, kc * 128:(kc + 1) * 128], ident[:],
                     sem=s_t if kc % 4 == 3 else None)
            if kc % 4 == 3:
                for k2 in range(kc - 3, kc + 1):
                    src = ps[:, bk, (k2 % 4) * 128:(k2 % 4 + 1) * 128]
                    dst = hT[:, k2, tt_ * 128:(tt_ + 1) * 128]
                    if (kc // 4) % 2 == 0:
                        p.wait("scalar", s_t, v)
                        ea_val[tt_] = p.act(dst, src, AF.Identity, sem=s_ea, scale=colv[:, SC1 + k2:SC1 + k2 + 1],
                                            bias=colv[:, SH1 + k2:SH1 + k2 + 1])
                    else:
                        p.wait("vector", s_t, v)
                        ev_val[tt_] = p.ts(dst, src, colv[:, SC1 + k2:SC1 + k2 + 1], colv[:, SH1 + k2:SH1 + k2 + 1],
                                           ALU.mult, ALU.add, sem=s_ev)
        tdone[tt_] = v
        if tt_ + 2 < 9:
            issue_x(tt_ + 2)
    if debug:
        p.wait("sync", s_ea)
        p.wait("sync", s_ev)
        p.dma("sync", dbg["d_hT"][:, :, :], hT[:], s_st)
        p.wait("sync", s_st)
    p.emit()
    free(g_xs)
    if upto == 1:
        return nc

    p = Prog(nc, es, "p2a")
    g_wk, wkd = sb("wkd", [128, KC, 4, 128], BF16)
    g_wv, wv = sb("wv", [128, KC, 256], BF16)
    g_wq, wq = sb("wq", [128, 3, KC, 128], BF16)
    s_w = p.sem("w")
    s_wq = p.semring("wq", 3)
    s_mm = p.sem("mm")
    s_ea = p.sem("ea")
    s_ev = p.sem("ev")
    kview = w_in[:, 1024:1280].rearrange("(kc p) (g i) -> p kc g i", p=128, i=64)
    for dup in range(2):
        for g in range(4):
            p.dma("gpsimd", wkd[:, :, g, dup * 64:(dup + 1) * 64], kview[:, :, g, :], s_w)
    p.dma("gpsimd", wv[:], w_in[:, 1280:1536].rearrange("(kc p) n -> p kc n", p=128), s_w)
    wqfull = {}
    wqfree = {}

    def issue_wq(c):
        if c >= 3:
            p.wait("gpsimd", s_mm, wqfree[c - 3])
        wqfull[c] = p.dma("gpsimd", wq[:, c % 3], w_in[:, c * 128:(c + 1) * 128].rearrange("(kc p) n -> p kc n", p=128), s_wq[c % 3])

    for c in range(3):
        issue_wq(c)
    ring = BankRing(p, range(8))
    evi = [0]

    def group(mms, n, dst):
        bk = ring.take()
        for i, (l, r) in enumerate(mms):
            v = p.mm(ps[:, bk, 0:n], l, r, i == 0, i == len(mms) - 1, sem=s_mm if i == len(mms) - 1 else None)
        evi[0] += 1
        if evi[0] % 2 == 0:
            p.wait("scalar", s_mm, v)
            ve = p.copy("scalar", dst, ps[:, bk, 0:n], sem=s_ea)
            ring.release(bk, s_ea, ve)
        else:
            p.wait("vector", s_mm, v)
            ve = p.copy("vector", dst, ps[:, bk, 0:n], sem=s_ev)
            ring.release(bk, s_ev, ve)
        return v

    p.wait("tensor", s_w)
    RANGES3 = [(0, 512), (512, 1024), (1024, 1152)]
    for g in range(4):
        for (a, b) in RANGES3:
            group([(wkd[:, kc, g, :], hT[:, kc, a:b]) for kc in range(KC)], b - a, kT[:, g, a:b])
    for tt_ in range(9):
        group([(hT[:, kc, tt_ * 128:(tt_ + 1) * 128], wv[:, kc, :]) for kc in range(KC)], 256, V[:, tt_, :])
    for c in range(8):
        p.wait("tensor", s_wq[c % 3], wqfull[c])
        for r in range(2):
            v = group([(wq[:, c % 3, kc, :], hT[:, kc, 128 + r * 512:128 + (r + 1) * 512]) for kc in range(KC)], 512,
                      qT[:, c, r * 512:(r + 1) * 512])
        wqfree[c] = v
        if c + 3 < 8:
            issue_wq(c + 3)
    p.emit()
    for g in (g_wq, g_wv, g_wk):
        free(g)
    if upto == 2:
        return nc

    p = Prog(nc, es, "p2b")
    g_wc, wc = sb("wc", [128, 2, KC, 3, 128], BF16)
    g_cw, cw = sb("cw", [128, 8, 3], F32)
    g_u, u_sb = sb("u_sb", [128, 2, 2 + TOWN], F32)
    g_chs, chs = sb("chs", [128, 2, 512], F32)
    g_chh, chh = sb("chh", [128, 2, 2], F32)
    g_y, ybuf = sb("ybuf", [128, 2, 512], F32)
    s_w = p.semring("w", 2)
    s_ld = p.sem("ld")
    s_mm = p.sem("mm")
    s_ea = p.sem("ea")
    s_ev = p.sem("ev")
    p.dma("sync", cw[:], convw[:, :, :], s_ld)
    cview = w_in[:, 1536:4608].rearrange("(kc p) (s c i) -> p kc s c i", p=128, s=3, c=8)
    wcfull = {}
    wcfree = {}

    def issue_wc(j):
        if j >= 2:
            p.wait("gpsimd", s_mm, wcfree[j - 2])
        for s3 in range(3):
            wcfull[j] = p.dma("gpsimd", wc[:, j % 2, :, s3, :], cview[:, :, s3, j, :], s_w[j % 2])

    issue_wc(0)
    issue_wc(1)
    p.wait("vector", s_ld)
    ringH = BankRing(p, [6, 7])
    ring3 = BankRing(p, [0, 1, 2, 3, 4, 5])
    CB, CCI, CH = 0, 1, 2
    k_ = 0
    for j in range(8):
        sl = j % 2
        p.wait("tensor", s_w[j % 2], wcfull[j])
        bh = ringH.take()
        for kc in range(KC):
            p.mm(ps[:, bh, 0:2], wc[:, sl, kc, CCI, :], hT[:, kc, 126:128], kc == 0, kc == KC - 1)
        for kc in range(KC):
            v = p.mm(ps[:, bh, 2:4], wc[:, sl, kc, CH, :], hT[:, kc, 126:128], kc == 0, kc == KC - 1,
                     sem=s_mm if kc == KC - 1 else None)
        p.wait("scalar", s_mm, v)
        va = p.copy("scalar", chh[:, sl, :], ps[:, bh, 2:4], sem=s_ea)
        p.wait("vector", s_ea, va)
        p.seq("vector")
        ve = p.stt(u_sb[:, sl, 0:2], ps[:, bh, 0:2], flg[:, 1:2], chh[:, sl, :], ALU.mult, ALU.mult, sem=s_ev)
        ringH.release(bh, s_ev, ve)
        for r in range(2):
            a, b = 128 + r * 512, 128 + (r + 1) * 512
            bcc, bch, bcb = ring3.take(), ring3.take(), ring3.take()
            for kc in range(KC):
                p.mm(bank(bcc), wc[:, sl, kc, CCI, :], hT[:, kc, a:b], kc == 0, kc == KC - 1)
            for kc in range(KC):
                vch = p.mm(bank(bch), wc[:, sl, kc, CH, :], hT[:, kc, a:b], kc == 0, kc == KC - 1,
                           sem=s_mm if kc == KC - 1 else None)
            for kc in range(KC):
                vcb = p.mm(bank(bcb), wc[:, sl, kc, CB, :], hT[:, kc, a:b], kc == 0, kc == KC - 1,
                           sem=s_mm if kc == KC - 1 else None)
            cs_ = k_ % 2
            k_ += 1
            p.wait("scalar", s_mm, vch)
            p.wait("scalar", s_ev, ve)
            va = p.copy("scalar", chs[:, cs_, :], bank(bch), sem=s_ea)
            ring3.release(bch, s_ea, va)
            p.wait("vector", s_ea, va)
            p.seq("vector")
            uo = 2 + r * 512
            ve = p.tt(u_sb[:, sl, uo:uo + 512], bank(bcc), chs[:, cs_, :], ALU.mult, sem=s_ev)
            ring3.release(bcc, s_ev, ve)
            p.seq("vector")
            p.ts(ybuf[:, cs_, :], u_sb[:, sl, uo - 2:uo + 510], cw[:, j, 0:1], None, ALU.mult, sem=s_ev)
            p.seq("vector")
            p.stt(ybuf[:, cs_, :], u_sb[:, sl, uo - 1:uo + 511], cw[:, j, 1:2], ybuf[:, cs_, :], ALU.mult, ALU.add, sem=s_ev)
            p.seq("vector")
            p.stt(ybuf[:, cs_, :], u_sb[:, sl, uo:uo + 512], cw[:, j, 2:3], ybuf[:, cs_, :], ALU.mult, ALU.add, sem=s_ev)
            p.seq("vector")
            p.wait("vector", s_mm, vcb)
            ve = p.tt(catT[:, 8 + j, r * 512:(r + 1) * 512], bank(bcb), ybuf[:, cs_, :], ALU.mult, sem=s_ev)
            ring3.release(bcb, s_ev, ve)
        wcfree[j] = vcb
        if j + 2 < 8:
            issue_wc(j + 2)
    p.emit()
    for g in (g_y, g_chh, g_chs, g_u, g_cw, g_wc):
        free(g)
    free(g_h)
    if upto == 3:
        return nc

    p = Prog(nc, es, "p3")
    g_ab, ab = sb("ab", [128, 4, 2, 512], F32)
    g_es, esink = sb("esink", [128, 8], F32)
    g_tmp, tmp = sb("tmp", [128, 2, 2, 512], F32)
    g_pt, PT = sb("PT", [128, 2, 2, 512], BF16)
    g_dt, dtmp = sb("dtmp", [128, 2, 256], F32)
    s_ld = p.sem("ld")
    s_s = p.sem("s")
    s_ev = p.sem("ev")
    s_ea = p.sem("ea")
    s_o = p.sem("o")
    s_n = p.sem("n")
    p.dma("sync", ab[:], abias[:, :, :, :], s_ld)
    p.dma("sync", esink[:], sinks[:, :], s_ld)
    p.wait("scalar", s_ld)
    v_es = p.act(esink[:], esink[:], AF.Exp, sem=s_ea)
    p.wait("vector", s_ld)
    p.wait("vector", s_ea, v_es)
    ringS = BankRing(p, [0, 1, 2, 3])
    ringO = BankRing(p, [4, 5])
    pt_free = {}
    act_done3 = {}
    exp_done = {}

    def stage_a(un, qb, g):
        up = un % 2
        sbanks = [ringS.take(), ringS.take()]
        for par in range(2):
            po = 64 * par
            for kb in range(2):
                kt = qb + kb
                for hh in range(2):
                    c = 2 * g + hh
                    col = (kb * 2 + hh) * 128
                    v = p.mm(ps[:, sbanks[par], col:col + 128], kT[po:po + 64, g, kt * 128:(kt + 1) * 128],
                             qT[po:po + 64, c, qb * 128:(qb + 1) * 128], True, True,
                             sem=s_s if (kb == 1 and hh == 1) else None)
            p.wait("vector", s_s, v)
            if un >= 2:
                p.wait("vector", s_ea, act_done3[(un - 2, par)])
            ve = p.stt(tmp[:, up, par, :], bank(sbanks[par]), 0.125, ab[:, g, par, :], ALU.mult, ALU.add, sem=s_ev)
            ringS.release(sbanks[par], s_ev, ve)
            p.wait("scalar", s_ev, ve)
            if un >= 2:
                p.wait("scalar", s_o, pt_free[un - 2])
            if qb == 0:
                p.act(PT[:, up, par, 0:256], tmp[:, up, par, 0:256], AF.Exp, sem=s_ea, bias=flg[:, 0:1])
                va = p.act(PT[:, up, par, 256:512], tmp[:, up, par, 256:512], AF.Exp, sem=s_ea)
            else:
                va = p.act(PT[:, up, par, :], tmp[:, up, par, :], AF.Exp, sem=s_ea)
            act_done3[(un, par)] = va
        exp_done[un] = va

    def stage_b(un, qb, g):
        up = un % 2
        p.wait("tensor", s_ea, exp_done[un])
        bo = ringO.take()
        for which in range(2):
            for hh in range(2):
                col = which * 256 + hh * 128
                for par in range(2):
                    for kb in range(2):
                        kt = qb + kb
                        lhs = V[:, kt, g * 64:(g + 1) * 64] if which == 0 else onesb[:, :]
                        pc = (kb * 2 + hh) * 128
                        last = (which == 1 and hh == 1 and par == 1 and kb == 1)
                        v = p.mm(ps[par * 64:(par + 1) * 64, bo, col:col + 128], lhs, PT[:, up, par, pc:pc + 128],
                                 kb == 0, kb == 1, sem=s_o if last else None)
        pt_free[un] = v
        p.wait("vector", s_o, v)
        for hh in range(2):
            c = 2 * g + hh
            p.ts(dtmp[:, up, hh * 128:(hh + 1) * 128], ps[:, bo, 256 + hh * 128:256 + (hh + 1) * 128],
                 esink[:, c:c + 1], None, ALU.add, sem=s_n)
        p.seq("vector")
        p.op("vector", lambda e, up=up: e.reciprocal(out=dtmp[:, up, :], in_=dtmp[:, up, :]), s_n)
        p.seq("vector")
        for hh in range(2):
            c = 2 * g + hh
            ve = p.tt(catT[:, c, qb * 128:(qb + 1) * 128], ps[:, bo, hh * 128:(hh + 1) * 128],
                      dtmp[:, up, hh * 128:(hh + 1) * 128], ALU.mult, sem=s_n)
        ringO.release(bo, s_n, ve)

    g_wm2, wm2 = sb("wm2", [128, 2, KC, 512], BF16)
    g_mr2, modrow2 = sb("modrow2", [1, D], F32)
    g_bb, biasb = sb("biasb", [128, 2, D], F32)
    s_w2 = p.semring("w2", 2)
    s_m2 = p.sem("m2")
    s_e2 = p.sem("e2")
    s_b2 = p.sem("b2")
    s_bb = p.semring("bb", 2)
    wmv = w_mod.rearrange("(kc p) n -> p kc n", p=128)
    SEGS = [(2, g1p, 1.0, None), (3, sh2b, 0.0, 2), (4, sc2b, 1.0, 3), (5, g2p, 1.0, None)]
    sblocks = [(si, blk) for si in range(4) for blk in range(4)]
    w2full = {}
    w2free = {}
    bb_full = {}
    bb_free = {}
    sst = {"b6": None, "bc": None}

    def issue_w2(i):
        si, blk = sblocks[i]
        b = SEGS[si][0] * 4 + blk
        if i >= 2:
            p.wait("gpsimd", s_m2, w2free[i - 2])
        w2full[i] = p.dma("gpsimd", wm2[:, i % 2], wmv[:, :, b * 512:(b + 1) * 512], s_w2[i % 2])

    def issue_bb(si):
        seg = SEGS[si][0]
        if si >= 2:
            p.wait("sync", s_e2, bb_free[si - 2])
        bb_full[si] = p.dma("sync", biasb[:, si % 2, :], b_mod[0:1, seg * 2048:(seg + 1) * 2048].broadcast_to([128, D]), s_bb[si % 2])

    issue_w2(0)
    issue_w2(1)
    issue_bb(0)
    issue_bb(1)

    def block_step(i):
        si, blk = sblocks[i]
        seg, gdst, plus, colg = SEGS[si]
        p.wait("tensor", s_w2[i % 2], w2full[i])
        if sst["b6"] is not None:
            p.wait("tensor", *sst["b6"])
        for kc in range(KC):
            v = p.mm(ps[0:1, 6, :], cs_bf[:, kc:kc + 1], wm2[:, i % 2, kc, :], kc == 0, kc == KC - 1,
                     sem=s_m2 if kc == KC - 1 else None)
        v_row = v
        if colg is not None:
            if sst.get("b7") is not None:
                p.wait("tensor", *sst["b7"])
            for c4 in range(4):
                idx = (colg - 2) * 16 + blk * 4 + c4
                for kc in range(KC):
                    v = p.mm(ps[:, 7, idx:idx + 1], wm2[:, i % 2, kc, c4 * 128:(c4 + 1) * 128], cs_bf[:, kc:kc + 1],
                             kc == 0, kc == KC - 1, sem=s_m2 if (kc == KC - 1 and c4 == 3) else None)
        w2free[i] = v
        p.wait("vector", s_m2, v_row)
        if blk == 0 and sst["bc"] is not None:
            p.wait("vector", *sst["bc"])
        ve = p.ts(modrow2[0:1, blk * 512:(blk + 1) * 512], ps[0:1, 6, :], plus, None, ALU.add, sem=s_e2)
        sst["b6"] = (s_e2, ve)
        if i + 2 < len(sblocks):
            issue_w2(i + 2)

    def seg_end_step(si):
        seg, gdst, plus, colg = SEGS[si]
        p.wait("tensor", *sst["b6"])
        p.wait("vector", s_bb[si % 2], bb_full[si])
        ve = None
        for n in range(4):
            if ve is not None:
                p.wait("tensor", s_e2, ve)
            v = p.mm(bank(6), ones_row[0:1, :], modrow2[0:1, n * 512:(n + 1) * 512], True, True, sem=s_b2)
            p.wait("vector", s_b2, v)
            ve = p.tt(gdst[:, n * 512:(n + 1) * 512], bank(6), biasb[:, si % 2, n * 512:(n + 1) * 512], ALU.add, sem=s_e2)
        sst["b6"] = (s_e2, ve)
        sst["bc"] = (s_b2, v)
        bb_free[si] = ve
        if si + 2 < 4:
            issue_bb(si + 2)
        if colg is not None:
            c0 = 48 if colg == 2 else 64
            vb7 = p.tt(colv[:, colg * 16:(colg + 1) * 16], ps[:, 7, (colg - 2) * 16:(colg - 1) * 16], bmc[:, c0:c0 + 16], ALU.add, sem=s_e2)
            sst["b7"] = (s_e2, vb7)
            if colg == 3:
                p.seq("vector")
                p.ts(colv[:, 48:64], colv[:, 48:64], 1.0, None, ALU.add, sem=s_e2)

    side_steps = []
    for si in range(4):
        for blk in range(4):
            side_steps.append((block_step, si * 4 + blk))
        side_steps.append((seg_end_step, si))

    unit_list = [(qb, g) for qb in range(NT) for g in range(4)]
    stage_a(0, *unit_list[0])
    for un, (qb, g) in enumerate(unit_list):
        if un + 1 < len(unit_list):
            stage_a(un + 1, *unit_list[un + 1])
        stage_b(un, qb, g)
        if un % 2 == 1 and un >= 3 and side_steps:
            fn, arg = side_steps.pop(0)
            fn(arg)
    while side_steps:
        fn, arg = side_steps.pop(0)
        fn(arg)
    if debug:
        s_st = p.sem("st")
        p.wait("sync", s_n)
        p.dma("sync", dbg["d_cat"][:, :, :], catT[:], s_st)
        p.wait("sync", s_st)
    p.emit()
    for g in (g_bb, g_mr2, g_wm2):
        free(g)
    for g in (g_dt, g_pt, g_tmp, g_es, g_ab):
        free(g)
    for g in (g_v, g_k, g_q):
        free(g)
    if upto == 4:
        return nc

    g_R, R = sb("R", [128, NT, D], F32)
    p = Prog(nc, es, "p4a")
    g_wo, wo = sb("wo", [128, 2, KC, 512], BF16)
    g_xt, xt = sb("xt", [128, 3, 512], F32)
    s_w = p.semring("w", 2)
    s_x = p.semring("x", 3)
    s_mm = p.sem("mm")
    s_ev = p.sem("ev")
    wov = w_out.rearrange("(kc p) n -> p kc n", p=128)
    wofull = {}
    wofree = {}

    def issue_wo(n):
        if n >= 2:
            p.wait("gpsimd", s_mm, wofree[n - 2])
        wofull[n] = p.dma("gpsimd", wo[:, n % 2], wov[:, :, n * 512:(n + 1) * 512], s_w[n % 2])

    issue_wo(0)
    issue_wo(1)
    xfull = {}
    xfree = {}
    items = [(n, t) for n in range(4) for t in range(NT)]

    def issue_xt(i):
        n, t = items[i]
        if i >= 3:
            p.wait("sync", s_ev, xfree[i - 3])
        xfull[i] = p.dma("sync", xt[:, i % 3, :], xh[128 + t * 128:128 + (t + 1) * 128, n * 512:(n + 1) * 512], s_x[i % 3])

    for i in range(3):
        issue_xt(i)
    ring = BankRing(p, [0, 1, 2, 3])
    for i, (n, t) in enumerate(items):
        if t == 0:
            p.wait("tensor", s_w[n % 2], wofull[n])
        bk = ring.take()
        for kc in range(KC):
            v = p.mm(bank(bk), catT[:, kc, t * 128:(t + 1) * 128], wo[:, n % 2, kc, :], kc == 0, kc == KC - 1,
                     sem=s_mm if kc == KC - 1 else None)
        if t == NT - 1:
            wofree[n] = v
            if n + 2 < 4:
                issue_wo(n + 2)
        p.wait("vector", s_mm, v)
        dst = R[:, t, n * 512:(n + 1) * 512]
        ve = p.tt(dst, bank(bk), g1p[:, n * 512:(n + 1) * 512], ALU.mult, sem=s_ev)
        ring.release(bk, s_ev, ve)
        p.wait("vector", s_x[i % 3], xfull[i])
        p.seq("vector")
        xfree[i] = p.stt(dst, xt[:, i % 3, :], ALPHA, dst, ALU.mult, ALU.add, sem=s_ev)
        if i + 3 < len(items):
            issue_xt(i + 3)
    p.emit()
    for g in (g_xt, g_wo):
        free(g)
    free(g_cat)
    if upto == 5:
        return nc

    g_h2, h2T = sb("h2T", [128, KC, TOWN], BF16, side="right")
    p = Prog(nc, es, "p4b")
    g_lg, lng = sb("lng", [128, D], F32)
    g_lb, lnb = sb("lnb", [128, D], F32)
    g_wr, wr = sb("wr", [128, KC, NE], F32)
    g_rb, rbias = sb("rbias", [128, NE], F32)
    g_hf, h2f = sb("h2f", [128, 2, KC, 128], F32)
    g_st, stats = sb("stats", [128, 4, 6], F32)
    g_mv, mv = sb("mv", [128, 2], F32)
    g_rs, rstd = sb("rstd", [128, 1], F32)
    g_gt, gt = sb("gt", [128, 16, NE], F32)
    g_g8, g8 = sb("g8", [128, 10, 8], F32)
    g_hk, h2tok = sb("h2tok", [128, 3, D], BF16)
    g_ht, h2tmp = sb("h2tmp", [128, D], F32)
    g_cb, chb = sb("chb", [128, NT, NE], BF16)
    g_lt, ltri = sb("ltri_sb", [128, 128], BF16)
    g_o1, ones128 = sb("ones128", [128, 128], BF16)
    g_eb, ebase = sb("ebase_sb", [128, NE], F32)
    s_ld = p.sem("ld")
    s_v = p.sem("v")
    s_a = p.sem("a")
    s_t = p.sem("t")
    s_r = p.sem("r")
    s_st = p.sem("st")
    s_pl = p.sem("pl")
    s_sc = p.semring("sc", 3)
    s_ps = p.sem("ps")
    p.dma("sync", lng[:], ln1_g.broadcast_to([128, D]), s_ld)
    p.dma("sync", lnb[:], ln1_b.broadcast_to([128, D]), s_ld)
    p.dma("sync", wr[:], w_router.rearrange("(kc p) n -> p kc n", p=128), s_ld)
    p.dma("sync", rbias[:], router_bias.broadcast_to([128, NE]), s_ld)
    p.dma("sync", ebase[:], ebase_d[:, :], s_ld)
    s_lt = p.sem("lt")
    p.dma("gpsimd", ltri[:], ltri_d[:, :], s_lt)
    p.op("vector", lambda e: e.memset(ones128[:], 1.0))
    v_o128 = p.last["vector"]
    p.wait("vector", s_ld)
    p.wait("gpsimd", s_ld)
    p.wait("tensor", s_ld)
    p.wait("tensor", s_lt)
    p.wait("tensor", *v_o128)
    ringT = BankRing(p, [0, 1, 2, 3, 4, 5])
    ringL = BankRing(p, [6, 7])
    hf_free = {}
    sc_done = {}
    dest_ready = {}
    regs = {}

    def _mkreg(e):
        regs["b"] = e.alloc_register("bnd4b")
        e.reg_mov(regs["b"], NE * CAP - 1)

    p.raw("gpsimd", _mkreg)

    h2_ready = {}

    def scatter_tile(t):
        p.wait("gpsimd", s_v, h2_ready[t])
        for k in range(8):
            sc_done[t] = p.op("gpsimd", lambda e, t=t, k=k: e.indirect_dma_start(
                out=hd[:, :], out_offset=bass.IndirectOffsetOnAxis(ap=dest_i[:, t, k:k + 1].bitcast(U32), axis=0),
                in_=h2tok[:, t % 3, :], in_offset=None, bounds_check=regs["b"], oob_is_err=False), s_sc[t % 3], dma=True)

    x1_done = {0: layer_norm_tile(p, R[:, 0, :], lng, lnb, stats, mv, rstd, s_v, s_a, s_pl)}
    for t in range(NT):
        v_x1 = x1_done[t]
        if t + 1 < NT:
            x1_done[t + 1] = layer_norm_tile(p, R[:, t + 1, :], lng, lnb, stats, mv, rstd, s_v, s_a, s_pl)
        p.wait("sync", *v_x1)
        p.dma("sync", x1s[t * 128:(t + 1) * 128, :], R[:, t, :], s_st)
        if debug:
            p.dma("sync", dbg["d_x1"][t * 128:(t + 1) * 128, :], R[:, t, :], s_st)
        p.wait("tensor", *v_x1)
        hs = t % 2
        if t >= 2:
            p.wait("scalar", s_r, hf_free[t - 2][0])
            p.wait("scalar", s_v, hf_free[t - 2][1])
        for q4 in range(4):
            bk = ringT.take()
            for i in range(4):
                kc = q4 * 4 + i
                v = p.tr(ps[:, bk, i * 128:(i + 1) * 128], R[:, t, kc * 128:(kc + 1) * 128], ident[:],
                         sem=s_t if i == 3 else None)
            p.wait("scalar", s_t, v)
            for i in range(4):
                kc = q4 * 4 + i
                va = p.act(h2f[:, hs, kc, :], ps[:, bk, i * 128:(i + 1) * 128], AF.Identity, sem=s_a,
                           scale=colv[:, SC2 + kc:SC2 + kc + 1], bias=colv[:, SH2 + kc:SH2 + kc + 1])
            ringT.release(bk, s_a, va)
        p.wait("vector", s_a, va)
        v_cast = p.copy("vector", h2T[:, :, t * 128:(t + 1) * 128], h2f[:, hs, :, :], sem=s_v)
        p.wait("tensor", s_a, va)
        bl = ringL.take()
        for kc in range(KC):
            v = p.mm(ps[:, bl, 0:NE], h2f[:, hs, kc, :], wr[:, kc, :], kc == 0, kc == KC - 1,
                     sem=s_r if kc == KC - 1 else None)
        hf_free[t] = (v, v_cast)
        vg = gating_tile(p, ps[:, bl, 0:NE], rbias, gt, g8, G[:, t, 0:NE], s_r, v, s_v, s_a)
        ringL.release(bl, s_a, vg)
        chosen = gt[:, 5, :]
        dd, v1_, valid, aa, negd, Gv, oh = (gt[:, 8 + i, :] for i in range(7))
        top8 = g8[:, 8, :]

        def V_(fn):
            p.seq("vector")
            return p.op("vector", fn, s_v)

        v_ch = V_(lambda e, t=t: e.tensor_copy(out=chb[:, t, :], in_=chosen))
        p.wait("tensor", s_v, v_ch)
        bp = ringL.take()
        for tp in range(t):
            p.mm(ps[:, bp, 0:NE], ones128[:], chb[:, tp, :], tp == 0, False)
        v_pos = p.mm(ps[:, bp, 0:NE], ltri[:], chb[:, t, :], t == 0, True, sem=s_ps)
        p.wait("vector", s_ps, v_pos)
        pos_ps = ps[:, bp, 0:NE]
        V_(lambda e: e.tensor_tensor(out=dd, in0=pos_ps, in1=ebase[:], op=ALU.add))
        v_rel = V_(lambda e: e.tensor_scalar(out=v1_, in0=pos_ps, scalar1=CAP - 0.5, scalar2=None, op0=ALU.is_lt))
        ringL.release(bp, s_v, v_rel)
        V_(lambda e: e.tensor_tensor(out=valid, in0=v1_, in1=chosen, op=ALU.mult))
        V_(lambda e: e.tensor_scalar(out=aa, in0=dd, scalar1=-1.0, scalar2=BIG, op0=ALU.mult, op1=ALU.add))
        V_(lambda e: e.tensor_tensor(out=aa, in0=aa, in1=valid, op=ALU.mult))
        V_(lambda e: e.tensor_scalar(out=negd, in0=aa, scalar1=-BIG, scalar2=None, op0=ALU.add))
        V_(lambda e: e.max(out=top8, in_=negd))
        dest_ready[t] = V_(lambda e, t=t: e.tensor_scalar(out=dest_i[:, t, :], in0=top8, scalar1=-1.0, scalar2=None, op0=ALU.mult))
        V_(lambda e, t=t: e.tensor_tensor(out=Gv, in0=G[:, t, 0:NE], in1=valid, op=ALU.mult))
        for k in range(8):
            V_(lambda e, k=k: e.scalar_tensor_tensor(out=oh, in0=negd, scalar=top8[:, k:k + 1], in1=Gv, op0=ALU.is_equal, op1=ALU.mult))
            V_(lambda e, t=t, k=k: e.tensor_reduce(out=wk[:, t, k:k + 1], in_=oh, axis=AX.X, op=ALU.add))
        p.wait("vector", *v_x1)
        if t >= 3:
            p.wait("vector", s_sc[t % 3], sc_done[t - 3])
        V_(lambda e, t=t: e.tensor_tensor(out=h2tmp[:], in0=R[:, t, :], in1=sc2b[:], op=ALU.mult))
        h2_ready[t] = V_(lambda e, t=t: e.tensor_tensor(out=h2tok[:, t % 3, :], in0=h2tmp[:], in1=sh2b[:], op=ALU.add))
        scatter_tile(t)
    g_pc, pcol = sb("pcol_sb", [128, NS], F32)
    g_ix, idxf = sb("idxf", [128, 3, NE], F32)
    s_pc = p.sem("pc")
    v_pc = p.dma("sync", pcol[:], pcol_d[:, :], s_pc)
    bc_ = ringL.take()
    for tp in range(NT):
        v_cnt = p.mm(ps[:, bc_, 0:NE], ones128[:], chb[:, tp, :], tp == 0, tp == NT - 1, sem=s_ps if tp == NT - 1 else None)
    p.wait("vector", s_ps, v_cnt)
    p.wait("vector", s_pc, v_pc)
    for st_ in range(NS):
        p.seq("vector")
        p.ts(idxf[:, 0, :], ps[:, bc_, 0:NE], pcol[:, st_:st_ + 1], None, ALU.is_gt, sem=s_v)
        p.ts(idxf[:, 1, :], ebase[:], pcol[:, st_:st_ + 1], -BIG, ALU.add, ALU.add, sem=s_v)
        p.seq("vector")
        p.tt(idxf[:, 2, :], idxf[:, 0, :], idxf[:, 1, :], ALU.mult, sem=s_v)
        p.seq("vector")
        p.ts(idx_i[:, st_, :], idxf[:, 2, :], BIG, None, ALU.add, sem=s_v)
    p.wait("sync", s_st)
    for q_ in s_sc:
        p.wait("gpsimd", q_)
    p.raw("gpsimd", lambda e: e.free_register(regs["b"]))
    if debug:
        p.seq("vector")
        p.wait("sync", *p.last["vector"])
        p.dma("sync", dbg["d_G"][:, :, :], G[:], s_st)
        p.dma("sync", dbg["d_dest"][:, :, :], dest_i[:], s_st)
        p.dma("sync", dbg["d_wk"][:, :, :], wk[:], s_st)
        p.wait("sync", s_st)
    p.emit()
    for g in (g_ix, g_pc, g_eb, g_o1, g_lt, g_cb, g_ht, g_hk, g_g8, g_gt, g_rs, g_mv, g_st, g_hf, g_rb, g_wr, g_lb, g_lg):
        free(g)
    free(g_R)
    if upto == 6:
        return nc

    p = Prog(nc, es, "p5a")
    g_gu, gu = sb("gu", [128, 3, 2, KC, 128], BF16)
    g_wd, wd = sb("wd", [128, 2, 4, D], BF16)
    g_sg, sg = sb("sg", [128, 2, 512], F32)
    g_at, actT = sb("actT", [128, 2, 4, TOWN], BF16)
    g_yb, ybuf = sb("ybuf", [128, 2, D], F32)
    s_gu = [p.sem("gu%d" % i) for i in range(3)]
    s_wd = p.sem("wd")
    s_mm = p.sem("mm")
    s_dn = p.sem("dn")
    s_a = p.sem("a")
    s_v = p.sem("v")
    s_y = p.semring("y", 2)
    gufull = {}
    gufree = {}

    def issue_gus(j):
        if j >= 3:
            p.wait("gpsimd", s_mm, gufree[j - 3])
        p.dma("gpsimd", gu[:, j % 3, 0], ws_gate.rearrange("(kc p) (j i) -> p kc j i", p=128, j=4)[:, :, j, :], s_gu[j % 3])
        gufull[j] = p.dma("gpsimd", gu[:, j % 3, 1], ws_up.rearrange("(kc p) (j i) -> p kc j i", p=128, j=4)[:, :, j, :], s_gu[j % 3])

    for j in range(3):
        issue_gus(j)
    v_wd = p.dma("gpsimd", wd[:, 0], ws_down.rearrange("(j p) n -> p j n", p=128), s_wd)
    ringGU = BankRing(p, [0, 1, 2, 3])
    ringD = BankRing(p, [4, 5, 6, 7])
    sg_k = 0
    sg_free = {}
    for j in range(4):
        p.wait("tensor", s_gu[j % 3], gufull[j])
        for th in range(2):
            bg, bu = ringGU.take(), ringGU.take()
            for kc in range(KC):
                vg_ = p.mm(bank(bg), gu[:, j % 3, 0, kc, :], h2T[:, kc, th * 512:(th + 1) * 512], kc == 0, kc == KC - 1,
                           sem=s_mm if kc == KC - 1 else None)
            for kc in range(KC):
                vu_ = p.mm(bank(bu), gu[:, j % 3, 1, kc, :], h2T[:, kc, th * 512:(th + 1) * 512], kc == 0, kc == KC - 1,
                           sem=s_mm if kc == KC - 1 else None)
            sk = sg_k % 2
            p.wait("scalar", s_mm, vg_)
            if sg_k >= 2:
                p.wait("scalar", s_v, sg_free[sg_k - 2])
            va = p.act(sg[:, sk, :], bank(bg), AF.Silu, sem=s_a)
            ringGU.release(bg, s_a, va)
            p.wait("vector", s_a, va)
            p.wait("vector", s_mm, vu_)
            vv = p.tt(actT[:, 0, j, th * 512:(th + 1) * 512], bank(bu), sg[:, sk, :], ALU.mult, sem=s_v)
            ringGU.release(bu, s_v, vv)
            sg_free[sg_k] = vv
            sg_k += 1
        gufree[j] = vu_
        if j + 3 < 4:
            issue_gus(j + 3)
    p.wait("tensor", s_wd, v_wd)
    p.wait("tensor", s_v, vv)
    ydma = {}
    for t in range(NT):
        yb = t % 2
        if t >= 2:
            p.wait("scalar", s_y[yb], ydma[t - 2])
        for n in range(4):
            bk = ringD.take()
            for j in range(4):
                v = p.mm(bank(bk), actT[:, 0, j, t * 128:(t + 1) * 128], wd[:, 0, j, n * 512:(n + 1) * 512],
                         j == 0, j == 3, sem=s_dn if j == 3 else None)
            p.wait("scalar", s_dn, v)
            va = p.act(ybuf[:, yb, n * 512:(n + 1) * 512], bank(bk), AF.Identity, sem=s_a)
            ringD.release(bk, s_a, va)
        p.wait("sync", s_a, va)
        ydma[t] = p.dma("sync", ysh[t * 128:(t + 1) * 128, :], ybuf[:, yb, :], s_y[yb])
    for q_ in s_y:
        p.wait("sync", q_)
    p.emit()
    for g in (g_yb, g_at, g_sg, g_wd, g_gu):
        free(g)
    free(g_h2)
    if upto == 7:
        return nc

    p = Prog(nc, es, "p5b")
    g_gu, gu = sb("gu", [128, 3, 2, KC, 256], BF16)
    g_wd, wd = sb("wd", [128, 2, 4, D], BF16)
    g_sg, sg = sb("sg", [128, 2, CAP], F32)
    g_at, actT = sb("actT", [128, 2, 4, CAP], BF16)
    g_yb, ybuf = sb("ybuf", [128, 3, D], F32)
    NSLOT = 4
    g_hs, hsel = sb("hsel", [128, NSLOT, D], BF16)
    g_hT, hselT = sb("hselT", [128, 2, KC, CAP], BF16)
    g_ib, identb = sb("identb", [128, 128], BF16)
    s_gu = [p.sem("gu%d" % i) for i in range(3)]
    s_hs = [p.sem("hs%d" % i) for i in range(NSLOT)]
    s_wd = p.semring("wd", 2)
    s_mm = p.sem("mm")
    s_dn = p.sem("dn")
    s_a = p.sem("a")
    s_v = p.sem("v")
    s_y = p.semring("y", 3)
    s_t = p.sem("t")
    s_ta = p.sem("ta")
    s_tv = p.sem("tv")
    s_ya = p.sem("ya")
    v_ib = p.copy("vector", identb[:], ident[:], sem=s_v)
    p.wait("tensor", s_v, v_ib)
    units = [(e, jp) for e in range(NE) for jp in range(2)]
    gufull = {}
    gufree = {}
    wdfull = {}
    wdfree = {}
    hsfull = {}
    hsfree = {}

    def issue_gu(i):
        e, jp = units[i]
        if i >= 3:
            p.wait("gpsimd", s_mm, gufree[i - 3])
        p.dma("gpsimd", gu[:, i % 3, 0], w_gate[e].rearrange("(kc p) (j i) -> p kc j i", p=128, j=2)[:, :, jp, :], s_gu[i % 3])
        gufull[i] = p.dma("gpsimd", gu[:, i % 3, 1], w_up[e].rearrange("(kc p) (j i) -> p kc j i", p=128, j=2)[:, :, jp, :], s_gu[i % 3])

    def issue_wd(e):
        if e >= 2:
            p.wait("gpsimd", s_dn, wdfree[e - 2])
        wdfull[e] = p.dma("gpsimd", wd[:, e % 2], w_down[e].rearrange("(j p) n -> p j n", p=128), s_wd[e % 2])

    regs5 = {}

    def _mkreg5(e):
        regs5["b"] = e.alloc_register("bnd5b")
        e.reg_mov(regs5["b"], NE * CAP - 1)

    p.raw("gpsimd", _mkreg5)
    p.op("vector", lambda e: e.memset(hsel[:, 0:2, :], 0.0))
    p.op("vector", lambda e: e.memset(hsel[:, 2:4, :], 0.0))
    p.wait("gpsimd", *p.last["vector"])

    def issue_hs(i):
        e_, st_ = divmod(i, NS)
        if i >= NSLOT:
            p.wait("gpsimd", s_t, hsfree[i - NSLOT])
        hsfull[i] = p.op("gpsimd", lambda e, e_=e_, st_=st_, i=i: e.indirect_dma_start(
            out=hsel[:, i % NSLOT, :], out_offset=None, in_=hd[:, :],
            in_offset=bass.IndirectOffsetOnAxis(ap=idx_i[:, st_, e_:e_ + 1].bitcast(U32), axis=0),
            bounds_check=regs5["b"], oob_is_err=False), s_hs[i % NSLOT], dma=True)

    for i in range(3):
        issue_gu(i)
    issue_wd(0)
    issue_wd(1)
    for i in range(NSLOT):
        issue_hs(i)
    ringGU = BankRing(p, [0, 1, 2, 3])
    ringD = BankRing(p, [4, 5])
    ringTr = BankRing(p, [6, 7])
    T_done = {}
    gu_done = {}
    act_done = {}
    d_done = {}
    ydma = {}
    ycnt = [0]
    sgk = [0]
    sg_free = {}

    def emit_T(e):
        eb = e % 2
        for st in range(NS):
            i = e * NS + st
            slot = i % NSLOT
            p.wait("tensor", s_hs[slot], hsfull[i])
            for half in range(2):
                bk = ringTr.take()
                psb = ps[:, bk, :].bitcast(BF16)
                for q in range(8):
                    kc = half * 8 + q
                    v = p.tr(psb[:, q * 128:(q + 1) * 128], hsel[:, slot, kc * 128:(kc + 1) * 128], identb[:],
                             sem=s_t if q == 7 else None)
                eng = "scalar" if half == 0 else "vector"
                sem = s_ta if half == 0 else s_tv
                p.wait(eng, s_t, v)
                if e >= 2 and st == 0:
                    p.wait(eng, s_mm, gu_done[e - 2])
                ve = p.copy(eng, hselT[:, eb, half * 8:(half + 1) * 8, st * 128:(st + 1) * 128],
                            psb[:, :].rearrange("p (q s) -> p q s", q=8), sem=sem)
                ringTr.release(bk, sem, ve)
                if half == 0:
                    va_ = ve
                else:
                    vv_ = ve
            hsfree[i] = v
            if i + NSLOT < NE * NS:
                issue_hs(i + NSLOT)
        T_done[e] = (va_, vv_)

    def emit_GU(e):
        eb = e % 2
        p.wait("tensor", s_ta, T_done[e][0])
        p.wait("tensor", s_tv, T_done[e][1])
        for j in range(4):
            i = e * 2 + j // 2
            jo = (j % 2) * 128
            if j % 2 == 0:
                p.wait("tensor", s_gu[i % 3], gufull[i])
            bg, bu = ringGU.take(), ringGU.take()
            for kc in range(KC):
                vg_ = p.mm(ps[:, bg, 0:CAP], gu[:, i % 3, 0, kc, jo:jo + 128], hselT[:, eb, kc, :], kc == 0, kc == KC - 1,
                           sem=s_mm if kc == KC - 1 else None)
            for kc in range(KC):
                vu_ = p.mm(ps[:, bu, 0:CAP], gu[:, i % 3, 1, kc, jo:jo + 128], hselT[:, eb, kc, :], kc == 0, kc == KC - 1,
                           sem=s_mm if kc == KC - 1 else None)
            sk = sgk[0] % 2
            p.wait("scalar", s_mm, vg_)
            if sgk[0] >= 2:
                p.wait("scalar", s_v, sg_free[sgk[0] - 2])
            va = p.act(sg[:, sk, :], ps[:, bg, 0:CAP], AF.Silu, sem=s_a)
            ringGU.release(bg, s_a, va)
            p.wait("vector", s_a, va)
            p.wait("vector", s_mm, vu_)
            if j == 0 and e >= 2:
                p.wait("vector", s_dn, d_done[e - 2])
            vv = p.tt(actT[:, eb, j, :], ps[:, bu, 0:CAP], sg[:, sk, :], ALU.mult, sem=s_v)
            ringGU.release(bu, s_v, vv)
            sg_free[sgk[0]] = vv
            sgk[0] += 1
            if j % 2 == 1:
                gufree[i] = vu_
                if i + 3 < len(units):
                    issue_gu(i + 3)
        gu_done[e] = vu_
        act_done[e] = vv

    def emit_D(e):
        eb = e % 2
        p.wait("tensor", s_wd[e % 2], wdfull[e])
        p.wait("tensor", s_v, act_done[e])
        for st in range(NS):
            u = ycnt[0]
            ycnt[0] += 1
            yb = u % 3
            if u >= 3:
                p.wait("scalar", s_y[yb], ydma[u - 3])
            for n in range(4):
                bk = ringD.take()
                for j in range(4):
                    v = p.mm(bank(bk), actT[:, eb, j, st * 128:(st + 1) * 128], wd[:, e % 2, j, n * 512:(n + 1) * 512],
                             j == 0, j == 3, sem=s_dn if j == 3 else None)
                p.wait("scalar", s_dn, v)
                va = p.act(ybuf[:, yb, n * 512:(n + 1) * 512], bank(bk), AF.Identity, sem=s_ya)
                ringD.release(bk, s_ya, va)
            p.wait("gpsimd", s_ya, va)
            ydma[u] = p.op("gpsimd", lambda e_, ex=e, st=st, yb=yb: e_.indirect_dma_start(
                out=yd[:, :], out_offset=bass.IndirectOffsetOnAxis(ap=idx_i[:, st, ex:ex + 1].bitcast(U32), axis=0),
                in_=ybuf[:, yb, :], in_offset=None, bounds_check=regs5["b"], oob_is_err=False), s_y[yb], dma=True)
        d_done[e] = v
        wdfree[e] = v
        if e + 2 < NE:
            issue_wd(e + 2)

    emit_T(0)
    for e in range(NE):
        emit_GU(e)
        if e + 1 < NE:
            emit_T(e + 1)
        emit_D(e)
    for q_ in s_y:
        p.wait("gpsimd", q_)
    p.raw("gpsimd", lambda e: e.free_register(regs5["b"]))
    p.emit()
    for g in (g_ib, g_hT, g_hs, g_yb, g_at, g_sg, g_wd, g_gu):
        free(g)
    if upto == 8:
        return nc

    p = Prog(nc, es, "p6")
    g_lg, lng = sb("lng2", [128, D], F32)
    g_lb, lnb = sb("lnb2", [128, D], F32)
    g_x1, x1t = sb("x1t", [128, 2, D], F32)
    g_ac, acct = sb("acct", [128, 2, D], F32)
    g_gb, gbuf = sb("gbuf", [128, 4, D], F32)
    g_st, stats = sb("stats2", [128, 4, 6], F32)
    g_mv, mv = sb("mv2", [128, 2], F32)
    g_rs, rstd = sb("rstd2", [128, 1], F32)
    s_ld = p.sem("ld")
    s_x = p.semring("x", 2)
    s_v = p.sem("v")
    s_a = p.sem("a")
    s_pl = p.sem("pl")
    s_st = p.semring("st", 2)
    s_g = p.semring("g", 4)
    p.dma("sync", lng[:], ln2_g.broadcast_to([128, D]), s_ld)
    p.dma("sync", lnb[:], ln2_b.broadcast_to([128, D]), s_ld)
    for i in range(4):
        p.op("vector", lambda e, i=i: e.memset(gbuf[:, i, :], 0.0))
    v_ms = p.last["vector"]
    p.wait("gpsimd", *v_ms)
    xfull = {}
    ydone = {}

    def issue_ld(t):
        if t >= 2:
            p.wait("sync", s_st[t % 2], ydone[t - 2])
        p.dma("sync", acct[:, t % 2, :], ysh[t * 128:(t + 1) * 128, :], s_x[t % 2])
        xfull[t] = p.dma("sync", x1t[:, t % 2, :], x1s[t * 128:(t + 1) * 128, :], s_x[t % 2])

    issue_ld(0)
    issue_ld(1)
    p.wait("vector", s_ld)
    gcnt = 0
    gfree = {}
    regs6 = {}

    def _mkreg6(e):
        regs6["b"] = e.alloc_register("bnd6")
        e.reg_mov(regs6["b"], NE * CAP - 1)

    p.raw("gpsimd", _mkreg6)
    for t in range(NT):
        a = t % 2
        acc = acct[:, a, :]
        p.wait("vector", s_x[t % 2], xfull[t])
        for k in range(8):
            gi = gcnt % 4
            if gcnt >= 4:
                p.wait("gpsimd", s_v, gfree[gcnt - 4])
            vg = p.op("gpsimd", lambda e, t=t, k=k, gi=gi: e.indirect_dma_start(
                out=gbuf[:, gi, :], out_offset=None, in_=yd[:, :],
                in_offset=bass.IndirectOffsetOnAxis(ap=dest_i[:, t, k:k + 1].bitcast(U32), axis=0),
                bounds_check=regs6["b"], oob_is_err=False), s_g[gi], dma=True)
            p.wait("vector", s_g[gi], vg)
            p.seq("vector")
            gfree[gcnt] = p.stt(acc, gbuf[:, gi, :], wk[:, t, k:k + 1], acc, ALU.mult, ALU.add, sem=s_v)
            gcnt += 1
        p.seq("vector")
        p.tt(acc, acc, g2p[:], ALU.mult, sem=s_v)
        p.seq("vector")
        p.stt(acc, x1t[:, a, :], ALPHA, acc, ALU.mult, ALU.add, sem=s_v)
        v_ln = layer_norm_tile(p, acc, lng, lnb, stats, mv, rstd, s_v, s_a, s_pl, gb_eng="vector")
        p.wait("sync", *v_ln)
        ydone[t] = p.dma("sync", y[t * 128:(t + 1) * 128, :], acc, s_st[t % 2])
        if t + 2 < NT:
            issue_ld(t + 2)
    for q_ in s_st:
        p.wait("sync", q_)
    p.emit()
    es.close()
    return nc


def layer_norm_tile(p, x, lng, lnb, stats, mv, rstd, s_v, s_a, s_pl, gb_eng="gpsimd"):
    p.seq("vector")
    for c in range(4):
        p.op("vector", lambda e, c=c: e.bn_stats(out=stats[:, c, :], in_=x[:, c * 512:(c + 1) * 512]), s_v)
    p.seq("vector")
    p.op("vector", lambda e: e.bn_aggr(out=mv[:], in_=stats[:].rearrange("p a b -> p (a b)")), s_v)
    p.seq("vector")
    v = p.ts(rstd[:], mv[:, 1:2], LN_EPS, None, ALU.add, sem=s_v)
    p.wait("scalar", s_v, v)
    va = p.act(rstd[:], rstd[:], AF.Sqrt, sem=s_a)
    p.wait("vector", s_a, va)
    p.op("vector", lambda e: e.reciprocal(out=rstd[:], in_=rstd[:]), s_v)
    p.seq("vector")
    vv = p.ts(x, x, mv[:, 0:1], rstd[:, 0:1], ALU.subtract, ALU.mult, sem=s_v)
    if gb_eng == "gpsimd":
        p.wait("gpsimd", s_v, vv)
        p.tt(x, x, lng[:], ALU.mult, sem=s_pl, eng="gpsimd")
        p.seq("gpsimd")
        v = p.tt(x, x, lnb[:], ALU.add, sem=s_pl, eng="gpsimd")
        return (s_pl, v)
    p.seq("vector")
    p.tt(x, x, lng[:], ALU.mult, sem=s_v)
    p.seq("vector")
    v = p.tt(x, x, lnb[:], ALU.add, sem=s_v)
    return (s_v, v)


def gating_tile(p, lg_ps, rbias, gt, g8, Gout, s_r, v_r, s_v, s_a):
    sc, sel, eq, sel2, selm, chosen, gw = (gt[:, i, :] for i in range(7))
    m1, m2, gs, rank, gmask, pen = (g8[:, i, :] for i in range(6))
    cmp = gt[:, 7, :]
    top8 = g8[:, 6, :]
    ssum = g8[:, 7, 0:1]
    rs = g8[:, 7, 1:2]

    def v3(ap):
        return ap.rearrange("p (g k) -> p g k", k=8)

    p.wait("scalar", s_r, v_r)
    p.seq("vector")
    p.wait("scalar", *p.last["vector"])
    va = p.act(sc, lg_ps, AF.Sigmoid, sem=s_a)
    p.wait("vector", s_a, va)

    def V_(fn):
        p.seq("vector")
        return p.op("vector", fn, s_v)

    V_(lambda e: e.tensor_tensor(out=sel, in0=sc, in1=rbias[:], op=ALU.add))
    V_(lambda e: e.tensor_reduce(out=m1, in_=v3(sel), axis=AX.X, op=ALU.max))
    V_(lambda e: e.tensor_tensor(out=v3(eq), in0=v3(sel), in1=m1.unsqueeze(2).broadcast_to([128, 8, 8]), op=ALU.is_equal))
    V_(lambda e: e.scalar_tensor_tensor(out=sel2, in0=eq, scalar=-1e9, in1=sel, op0=ALU.mult, op1=ALU.add))
    V_(lambda e: e.tensor_reduce(out=m2, in_=v3(sel2), axis=AX.X, op=ALU.max))
    V_(lambda e: e.tensor_tensor(out=gs, in0=m1, in1=m2, op=ALU.add))
    V_(lambda e: e.tensor_tensor(out=v3(cmp), in0=gs.unsqueeze(1).broadcast_to([128, 8, 8]),
                                 in1=gs.unsqueeze(2).broadcast_to([128, 8, 8]), op=ALU.is_gt))
    V_(lambda e: e.tensor_reduce(out=rank, in_=v3(cmp), axis=AX.X, op=ALU.add))
    V_(lambda e: e.tensor_scalar(out=gmask, in0=rank, scalar1=3.5, scalar2=None, op0=ALU.is_lt))
    V_(lambda e: e.tensor_scalar(out=pen, in0=gmask, scalar1=-1.0, scalar2=1e9, op0=ALU.add, op1=ALU.mult))
    V_(lambda e: e.tensor_tensor(out=v3(selm), in0=v3(sel), in1=pen.unsqueeze(2).broadcast_to([128, 8, 8]), op=ALU.add))
    V_(lambda e: e.max(out=top8, in_=selm))
    V_(lambda e: e.tensor_scalar(out=chosen, in0=selm, scalar1=top8[:, 7:8], scalar2=None, op0=ALU.is_ge))
    V_(lambda e: e.tensor_tensor(out=gw, in0=chosen, in1=sc, op=ALU.mult))
    V_(lambda e: e.tensor_reduce(out=ssum, in_=gw, axis=AX.X, op=ALU.add))
    V_(lambda e: e.reciprocal(out=rs, in_=ssum))
    V_(lambda e: e.tensor_scalar(out=Gout, in0=gw, scalar1=rs, scalar2=ROUTED_SCALE, op0=ALU.mult, op1=ALU.mult))
    return va


def _alibi_bias():
    slopes = (2.0 ** (-8.0 * np.arange(1, 17, dtype=np.float64) / 16)).astype(np.float32)
    k = np.arange(128)[:, None].astype(np.float32)
    q = np.arange(128)[None, :].astype(np.float32)
    out = np.zeros((128, 4, 2, 2, 2, 128), np.float32)
    for g in range(4):
        for par in range(2):
            for hh in range(2):
                h = 4 * g + 2 * hh + par
                dist_prev = q + 128 - k
                dist_cur = q - k
                out[:, g, par, 0, hh, :] = np.where(dist_prev < 128, -slopes[h] * dist_prev, NEG)
                out[:, g, par, 1, hh, :] = np.where(dist_cur >= 0, -slopes[h] * dist_cur, NEG)
    return out.reshape(128, 4, 2, 512)


_CACHE = {}


def _prep_inputs(inputs):
    f = lambda a: np.ascontiguousarray(np.asarray(a, dtype=np.float32))
    x = f(inputs["x"])
    c = f(inputs["c"])
    shared = {
        "w_mod": f(inputs["w_mod"][0]),
        "b_mod": f(inputs["b_mod"]).reshape(1, -1),
        "bmod_col": f(np.asarray(inputs["b_mod"]).reshape(96, 128).T),
        "w_in": f(inputs["w_in"][0]),
        "convw": f(np.asarray(inputs["conv_w"][0]).reshape(3, 8, 128).transpose(2, 1, 0)),
        "sinks": f(np.asarray(inputs["attn_sinks"][0]).reshape(8, 2)[:, :, None].repeat(64, axis=2).reshape(8, 128).T),
        "w_out": f(inputs["w_out"][0]),
        "ln1_g": f(inputs["ln1_g"]).reshape(1, -1),
        "ln1_b": f(inputs["ln1_b"]).reshape(1, -1),
        "ln2_g": f(inputs["ln2_g"]).reshape(1, -1),
        "ln2_b": f(inputs["ln2_b"]).reshape(1, -1),
        "w_router": f(inputs["w_router"][0]),
        "router_bias": f(inputs["router_bias"]).reshape(1, -1),
        "w_gate": f(inputs["w_gate"][0]),
        "w_up": f(inputs["w_up"][0]),
        "w_down": f(inputs["w_down"][0]),
        "ws_gate": f(inputs["ws_gate"][0]),
        "ws_up": f(inputs["ws_up"][0]),
        "ws_down": f(inputs["ws_down"][0]),
        "abias": _alibi_bias(),
        "ident_in": np.eye(128, dtype=np.float32),
        "ltri": np.triu(np.ones((128, 128), np.float32), k=1),
        "ebase": np.broadcast_to((np.arange(NE, dtype=np.float32) * CAP)[None, :], (128, NE)).copy(),
        "pcol": (np.arange(128, dtype=np.float32)[:, None] + 128.0 * np.arange(NS, dtype=np.float32)[None, :]).copy(),
    }
    in_maps = []
    for i in range(NCORES):
        b, ch = divmod(i, 4)
        t0 = ch * TOWN
        xh = np.zeros((TALL, D), np.float32)
        xh[128:] = x[b, t0:t0 + TOWN]
        flags = np.zeros((128, 2), np.float32)
        if ch > 0:
            xh[:128] = x[b, t0 - 128:t0]
            flags[:, 1] = 1.0
        else:
            flags[:, 0] = NEG
        m = dict(shared)
        m["xh"] = xh
        m["ccol"] = np.ascontiguousarray(c[b].reshape(KC, 128).T)
        m["flags"] = flags
        in_maps.append(m)
    return in_maps


def kernel(**inputs):
    if "nc" not in _CACHE:
        _CACHE["nc"] = build_program()
    nc = _CACHE["nc"]
    in_maps = _prep_inputs(inputs)
    res = run_bass_kernel_spmd(nc, in_maps, core_ids=list(range(NCORES)))
    out = np.empty((2, 4096, D), np.float32)
    for i in range(NCORES):
        b, ch = divmod(i, 4)
        out[b, ch * TOWN:(ch + 1) * TOWN] = res.results[i]["y"]
    return out
```

```python
import contextlib
import numpy as np
import concourse.bass as bass
import concourse.mybir as mybir
from concourse.bass_utils import run_bass_kernel_spmd

F32 = mybir.dt.float32
BF16 = mybir.dt.bfloat16
AF = mybir.ActivationFunctionType
ALU = mybir.AluOpType
AX = mybir.AxisListType

NCORES = 8
D = 2048
KC = 16
TOWN = 1024
TALL = 1152
NT = 8
INW = 4608
NE = 64
DEXP = 512
ALPHA = 2.0 ** 0.25
LN_EPS = 1e-5
ROUTED_SCALE = 2.5
NEG = -30000.0
CAP = 384
NS = CAP // 128
BIG = 1.0e6
I32 = mybir.dt.int32
U32 = mybir.dt.uint32


class Sem:
    def __init__(self, h):
        self.h = h
        self.n = 0


class Prog:
    ENG = ("tensor", "vector", "scalar", "gpsimd", "sync")

    def __init__(self, nc, es, tag):
        self.nc = nc
        self.es = es
        self.tag = tag
        self.q = {e: [] for e in self.ENG}
        self.last = {e: None for e in self.ENG}
        self.nsem = 0
        self.handles = []
        self.selfsem = {e: self.sem("self_" + e) for e in ("vector", "scalar", "gpsimd")}

    def sem(self, name=None):
        self.nsem += 1
        h = self.nc.alloc_semaphore(name=f"{self.tag}_{name or 's'}_{self.nsem}")
        self.handles.append(h)
        return Sem(h)

    def op(self, eng, fn, sem=None, dma=False):
        if sem is None and not dma and eng in self.selfsem:
            sem = self.selfsem[eng]
        if sem is None:
            self.q[eng].append(("op", fn, None, 0))
            return None
        inc = 16 if dma else 1
        sem.n += inc
        self.q[eng].append(("op", fn, sem, inc))
        if not dma:
            self.last[eng] = (sem, sem.n)
        return sem.n

    def raw(self, eng, fn):
        self.q[eng].append(("raw", fn))

    def semring(self, name, n):
        return [self.sem(f"{name}{i}") for i in range(n)]

    def wait(self, eng, sem, val=None):
        if val is None:
            val = sem.n
        assert val <= sem.n, (eng, val, sem.n)
        if val <= 0:
            return
        self.q[eng].append(("wait", sem, val))

    def seq(self, eng):
        if self.last[eng] is not None:
            self.wait(eng, *self.last[eng])

    def emit(self):
        with self.nc.Block() as block:
            for eng in self.ENG:
                items = self.q[eng]
                if not items:
                    continue

                def body(e, items=items):
                    for it in items:
                        if it[0] == "wait":
                            e.wait_ge(it[1].h, it[2])
                        elif it[0] == "raw":
                            it[1](e)
                        else:
                            ins = it[1](e)
                            if it[2] is not None:
                                ins.then_inc(it[2].h, it[3])

                getattr(block, eng)(body)
        self.nc.clear_and_free_semaphores(self.handles)
        self.nc.all_engine_barrier()

    def mm(self, out, lhsT, rhs, start, stop, sem=None):
        return self.op("tensor", lambda e: e.matmul(out, lhsT=lhsT, rhs=rhs, start=start, stop=stop), sem)

    def tr(self, out, in_, ident, sem=None):
        return self.op("tensor", lambda e: e.transpose(out=out, in_=in_, identity=ident), sem)

    def act(self, out, in_, func, sem=None, **kw):
        return self.op("scalar", lambda e: e.activation(out=out, in_=in_, func=func, **kw), sem)

    def tt(self, out, in0, in1, op, sem=None, eng="vector"):
        return self.op(eng, lambda e: e.tensor_tensor(out=out, in0=in0, in1=in1, op=op), sem)

    def ts(self, out, in0, s1, s2, op0, op1=None, sem=None, eng="vector"):
        if op1 is None:
            return self.op(eng, lambda e: e.tensor_scalar(out=out, in0=in0, scalar1=s1, scalar2=None, op0=op0), sem)
        return self.op(eng, lambda e: e.tensor_scalar(out=out, in0=in0, scalar1=s1, scalar2=s2, op0=op0, op1=op1), sem)

    def stt(self, out, in0, scalar, in1, op0, op1, sem=None):
        return self.op("vector", lambda e: e.scalar_tensor_tensor(out=out, in0=in0, scalar=scalar, in1=in1, op0=op0, op1=op1), sem)

    def copy(self, eng, out, in_, sem=None):
        if eng == "scalar":
            return self.op("scalar", lambda e: e.copy(out=out, in_=in_), sem)
        return self.op(eng, lambda e: e.tensor_copy(out=out, in_=in_), sem)

    def dma(self, eng, out, in_, sem):
        return self.op(eng, lambda e: e.dma_start(out=out, in_=in_), sem, dma=True)


class BankRing:
    def __init__(self, p, banks):
        self.p = p
        self.banks = list(banks)
        self.i = 0
        self.cond = {b: [] for b in self.banks}

    def take(self):
        b = self.banks[self.i % len(self.banks)]
        self.i += 1
        for (s, v) in self.cond[b]:
            self.p.wait("tensor", s, v)
        self.cond[b] = []
        return b

    def release(self, b, sem, val):
        self.cond[b].append((sem, val))


_DECLARED = []


def build_program(debug=False, upto=99):
    nc = bass.Bass("TRN2", target_bir_lowering=False)
    es = contextlib.ExitStack()

    _DECLARED.clear()

    def din(name, shape, dt=F32):
        if (upto < 7 and name in ("ws_gate", "ws_up", "ws_down")) or (upto < 8 and name in ("w_gate", "w_up", "w_down")):
            return None
        _DECLARED.append(name)
        return nc.dram_tensor(name, list(shape), dt, kind="ExternalInput").ap()

    xh = din("xh", [TALL, D])
    ccol = din("ccol", [128, KC])
    w_mod = din("w_mod", [D, 6 * D])
    b_mod = din("b_mod", [1, 6 * D])
    bmod_col = din("bmod_col", [128, 96])
    w_in = din("w_in", [D, INW])
    convw = din("convw", [128, 8, 3])
    sinks = din("sinks", [128, 8])
    w_out = din("w_out", [D, D])
    ln1_g = din("ln1_g", [1, D])
    ln1_b = din("ln1_b", [1, D])
    ln2_g = din("ln2_g", [1, D])
    ln2_b = din("ln2_b", [1, D])
    w_router = din("w_router", [D, NE])
    router_bias = din("router_bias", [1, NE])
    w_gate = din("w_gate", [NE, D, DEXP])
    w_up = din("w_up", [NE, D, DEXP])
    w_down = din("w_down", [NE, DEXP, D])
    ws_gate = din("ws_gate", [D, DEXP])
    ws_up = din("ws_up", [D, DEXP])
    ws_down = din("ws_down", [DEXP, D])
    abias = din("abias", [128, 4, 2, 512])
    flags = din("flags", [128, 2])
    ident_d = din("ident_in", [128, 128])
    y = nc.dram_tensor("y", [TOWN, D], F32, kind="ExternalOutput").ap()
    x1s = nc.dram_tensor("x1s", [TOWN, D], F32, kind="Internal").ap()
    ysh = nc.dram_tensor("ysh", [TOWN, D], F32, kind="Internal").ap()
    hd = nc.dram_tensor("hd", [NE * CAP, D], BF16, kind="Internal").ap()
    yd = nc.dram_tensor("yd", [NE * CAP, D], F32, kind="Internal").ap()
    ltri_d = din("ltri", [128, 128])
    ebase_d = din("ebase", [128, NE])
    pcol_d = din("pcol", [128, NS])
    dbg = {}
    if debug:
        dbg["d_hT"] = nc.dram_tensor("d_hT", [128, KC, TALL], BF16, kind="ExternalOutput").ap()
        dbg["d_cat"] = nc.dram_tensor("d_cat", [128, KC, TOWN], BF16, kind="ExternalOutput").ap()
        dbg["d_x1"] = nc.dram_tensor("d_x1", [TOWN, D], F32, kind="ExternalOutput").ap()
        dbg["d_G"] = nc.dram_tensor("d_G", [128, NT, NE + 1], F32, kind="ExternalOutput").ap()
        dbg["d_colv"] = nc.dram_tensor("d_colv", [128, 64], F32, kind="ExternalOutput").ap()
        dbg["d_g1p"] = nc.dram_tensor("d_g1p", [128, D], F32, kind="ExternalOutput").ap()
        dbg["d_dest"] = nc.dram_tensor("d_dest", [128, NT, 8], I32, kind="ExternalOutput").ap()
        dbg["d_wk"] = nc.dram_tensor("d_wk", [128, NT, 8], F32, kind="ExternalOutput").ap()

    _names = {}

    def sb(name, shape, dt, side="left"):
        _names[name] = _names.get(name, 0) + 1
        if _names[name] > 1:
            name = f"{name}_{_names[name]}"
        g = nc.sbuf_tensor(name, list(shape), dt, side=side)
        t = g.__enter__()
        return g, t

    def free(g):
        g.__exit__(None, None, None)

    _, ident = sb("ident", [128, 128], F32)
    _, colv = sb("colv", [128, 64], F32)
    _, g1p = sb("g1p", [128, D], F32)
    _, g2p = sb("g2p", [128, D], F32)
    _, flg = sb("flg", [128, 2], F32)
    _, onesb = sb("onesb", [128, 64], BF16)
    _, G = sb("G", [128, NT, NE + 1], F32)
    _, sh2b = sb("sh2b", [128, D], F32)
    _, sc2b = sb("sc2b", [128, D], F32)
    _, dest_i = sb("dest_i", [128, NT, 8], I32)
    _, wk = sb("wk", [128, NT, 8], F32)
    _, idx_i = sb("idx_i", [128, NS, NE], I32)
    _, cs_bf = sb("cs_bf", [128, KC], BF16)
    _, ones_row = sb("ones_row", [1, 128], F32)
    _, bmc = sb("bmc", [128, 96], F32)
    ps_g = nc.psum_tensor("ps", [128, 8, 512], F32)
    ps = ps_g.__enter__()

    def bank(i):
        return ps[:, i, :]

    SH1, SC1, SH2, SC2 = 0, 16, 32, 48

    p = Prog(nc, es, "p0")
    g_cc, cc_t = sb("cc_t", [128, KC], F32)
    g_wm, wm = sb("wm", [128, 2, KC, 512], BF16)
    s_ld = p.sem("ld")
    s_w = p.semring("w", 2)
    s_mm = p.sem("mm")
    s_act = p.sem("act")
    p.dma("sync", cc_t[:], ccol[:, :], s_ld)
    p.dma("sync", bmc[:], bmod_col[:, :], s_ld)
    p.dma("sync", ident[:], ident_d[:, :], s_ld)
    p.dma("sync", flg[:], flags[:, :], s_ld)
    p.wait("scalar", s_ld)
    v_cs = p.act(cs_bf[:], cc_t[:], AF.Silu, sem=s_act)
    p.op("vector", lambda e: e.memset(ones_row[:], 1.0))
    p.op("vector", lambda e: e.memset(onesb[:], 1.0))
    p.op("vector", lambda e: e.memset(G[:], 1.0))
    wmv = w_mod.rearrange("(kc p) n -> p kc n", p=128)
    NB0 = 8
    wfull = {}
    wfree = {}

    def issue_wm(b):
        if b >= 2:
            p.wait("gpsimd", s_mm, wfree[b - 2])
        wfull[b] = p.dma("gpsimd", wm[:, b % 2], wmv[:, :, b * 512:(b + 1) * 512], s_w[b % 2])

    issue_wm(0)
    issue_wm(1)
    p.wait("tensor", s_act, v_cs)
    p.wait("vector", s_ld)
    for b in range(NB0):
        p.wait("tensor", s_w[b % 2], wfull[b])
        seg = b // 4
        for c4 in range(4):
            idx = seg * 16 + (b % 4) * 4 + c4
            for kc in range(KC):
                v = p.mm(ps[:, 2, idx:idx + 1], wm[:, b % 2, kc, c4 * 128:(c4 + 1) * 128], cs_bf[:, kc:kc + 1],
                         kc == 0, kc == KC - 1, sem=s_mm if (kc == KC - 1 and c4 == 3) else None)
        wfree[b] = v
        if b + 2 < NB0:
            issue_wm(b + 2)
    p.wait("vector", s_mm, v)
    for gi, c0 in enumerate((0, 16)):
        p.tt(colv[:, gi * 16:(gi + 1) * 16], ps[:, 2, gi * 16:(gi + 1) * 16], bmc[:, c0:c0 + 16], ALU.add)
    p.seq("vector")
    p.ts(colv[:, 16:32], colv[:, 16:32], 1.0, None, ALU.add)
    p.emit()
    for g in (g_wm, g_cc):
        free(g)
    if upto == 0:
        return nc

    g_cat, catT = sb("catT", [128, KC, TOWN], BF16, side="right")
    g_q, qT = sb("qT", [128, 8, TOWN], BF16)
    g_k, kT = sb("kT", [128, 4, TALL], BF16)
    g_v, V = sb("V", [128, 9, 256], BF16)
    g_h, hT = sb("hT", [128, KC, TALL], BF16)

    p = Prog(nc, es, "p1")
    g_xs, xs = sb("xs", [128, 2, D], F32)
    s_x = p.semring("x", 2)
    s_t = p.sem("t")
    s_ea = p.sem("ea")
    s_ev = p.sem("ev")
    s_st = p.sem("st")
    xfull = {}
    tdone = {}

    def issue_x(tt_):
        if tt_ >= 2:
            p.wait("sync", s_t, tdone[tt_ - 2])
        xfull[tt_] = p.dma("sync", xs[:, tt_ % 2, :], xh[tt_ * 128:(tt_ + 1) * 128, :], s_x[tt_ % 2])

    issue_x(0)
    issue_x(1)
    ea_val = {}
    ev_val = {}
    for tt_ in range(9):
        p.wait("tensor", s_x[tt_ % 2], xfull[tt_])
        if tt_ >= 2:
            p.wait("tensor", s_ea, ea_val[tt_ - 2])
            p.wait("tensor", s_ev, ev_val[tt_ - 2])
        for kc in range(KC):
            bk = (tt_ % 2) * 4 + kc // 4
            v = p.tr(ps[:, bk, (kc % 4) * 128:(kc % 4 + 1) * 128], xs[:, tt_ % 2, kc * 128:(kc + 1) * 128], ident[:],
                     sem=s_t if kc % 4 == 3 else None)
            if kc % 4 == 3:
                for k2 in range(kc - 3, kc + 1):
                    src = ps[:, bk, (k2 % 4) * 128:(k2 % 4 + 1) * 128]
                    dst = hT[:, k2, tt_ * 128:(tt_ + 1) * 128]
                    if (kc // 4) % 2 == 0:
                        p.wait("scalar", s_t, v)
                        ea_val[tt_] = p.act(dst, src, AF.Identity, sem=s_ea, scale=colv[:, SC1 + k2:SC1 + k2 + 1],
                                            bias=colv[:, SH1 + k2:SH1 + k2 + 1])
                    else:
                        p.wait("vector", s_t, v)
                        ev_val[tt_] = p.ts(dst, src, colv[:, SC1 + k2:SC1 + k2 + 1], colv[:, SH1 + k2:SH1 + k2 + 1],
                                           ALU.mult, ALU.add, sem=s_ev)
        tdone[tt_] = v
        if tt_ + 2 < 9:
            issue_x(tt_ + 2)
    if debug:
        p.wait("sync", s_ea)
        p.wait("sync", s_ev)
        p.dma("sync", dbg["d_hT"][:, :, :], hT[:], s_st)
        p.wait("sync", s_st)
    p.emit()
    free(g_xs)
    if upto == 1:
        return nc

    p = Prog(nc, es, "p2a")
    g_wk, wkd = sb("wkd", [128, KC, 4, 128], BF16)
    g_wv, wv = sb("wv", [128, KC, 256], BF16)
    g_wq, wq = sb("wq", [128, 3, KC, 128], BF16)
    s_w = p.sem("w")
    s_wq = p.semring("wq", 3)
    s_mm = p.sem("mm")
    s_ea = p.sem("ea")
    s_ev = p.sem("ev")
    kview = w_in[:, 1024:1280].rearrange("(kc p) (g i) -> p kc g i", p=128, i=64)
    for dup in range(2):
        for g in range(4):
            p.dma("gpsimd", wkd[:, :, g, dup * 64:(dup + 1) * 64], kview[:, :, g, :], s_w)
    p.dma("gpsimd", wv[:], w_in[:, 1280:1536].rearrange("(kc p) n -> p kc n", p=128), s_w)
    wqfull = {}
    wqfree = {}

    def issue_wq(c):
        if c >= 3:
            p.wait("gpsimd", s_mm, wqfree[c - 3])
        wqfull[c] = p.dma("gpsimd", wq[:, c % 3], w_in[:, c * 128:(c + 1) * 128].rearrange("(kc p) n -> p kc n", p=128), s_wq[c % 3])

    for c in range(3):
        issue_wq(c)
    ring = BankRing(p, range(8))
    evi = [0]

    def group(mms, n, dst):
        bk = ring.take()
        for i, (l, r) in enumerate(mms):
            v = p.mm(ps[:, bk, 0:n], l, r, i == 0, i == len(mms) - 1, sem=s_mm if i == len(mms) - 1 else None)
        evi[0] += 1
        if evi[0] % 2 == 0:
            p.wait("scalar", s_mm, v)
            ve = p.copy("scalar", dst, ps[:, bk, 0:n], sem=s_ea)
            ring.release(bk, s_ea, ve)
        else:
            p.wait("vector", s_mm, v)
            ve = p.copy("vector", dst, ps[:, bk, 0:n], sem=s_ev)
            ring.release(bk, s_ev, ve)
        return v

    p.wait("tensor", s_w)
    RANGES3 = [(0, 512), (512, 1024), (1024, 1152)]
    for g in range(4):
        for (a, b) in RANGES3:
            group([(wkd[:, kc, g, :], hT[:, kc, a:b]) for kc in range(KC)], b - a, kT[:, g, a:b])
    for tt_ in range(9):
        group([(hT[:, kc, tt_ * 128:(tt_ + 1) * 128], wv[:, kc, :]) for kc in range(KC)], 256, V[:, tt_, :])
    for c in range(8):
        p.wait("tensor", s_wq[c % 3], wqfull[c])
        for r in range(2):
            v = group([(wq[:, c % 3, kc, :], hT[:, kc, 128 + r * 512:128 + (r + 1) * 512]) for kc in range(KC)], 512,
                      qT[:, c, r * 512:(r + 1) * 512])
        wqfree[c] = v
        if c + 3 < 8:
            issue_wq(c + 3)
    p.emit()
    for g in (g_wq, g_wv, g_wk):
        free(g)
    if upto == 2:
        return nc

    p = Prog(nc, es, "p2b")
    g_wc, wc = sb("wc", [128, 2, KC, 3, 128], BF16)
    g_cw, cw = sb("cw", [128, 8, 3], F32)
    g_u, u_sb = sb("u_sb", [128, 2, 2 + TOWN], F32)
    g_chs, chs = sb("chs", [128, 2, 512], F32)
    g_chh, chh = sb("chh", [128, 2, 2], F32)
    g_y, ybuf = sb("ybuf", [128, 2, 512], F32)
    s_w = p.semring("w", 2)
    s_ld = p.sem("ld")
    s_mm = p.sem("mm")
    s_ea = p.sem("ea")
    s_ev = p.sem("ev")
    p.dma("sync", cw[:], convw[:, :, :], s_ld)
    cview = w_in[:, 1536:4608].rearrange("(kc p) (s c i) -> p kc s c i", p=128, s=3, c=8)
    wcfull = {}
    wcfree = {}

    def issue_wc(j):
        if j >= 2:
            p.wait("gpsimd", s_mm, wcfree[j - 2])
        for s3 in range(3):
            wcfull[j] = p.dma("gpsimd", wc[:, j % 2, :, s3, :], cview[:, :, s3, j, :], s_w[j % 2])

    issue_wc(0)
    issue_wc(1)
    p.wait("vector", s_ld)
    ringH = BankRing(p, [6, 7])
    ring3 = BankRing(p, [0, 1, 2, 3, 4, 5])
    CB, CCI, CH = 0, 1, 2
    k_ = 0
    for j in range(8):
        sl = j % 2
        p.wait("tensor", s_w[j % 2], wcfull[j])
        bh = ringH.take()
        for kc in range(KC):
            p.mm(ps[:, bh, 0:2], wc[:, sl, kc, CCI, :], hT[:, kc, 126:128], kc == 0, kc == KC - 1)
        for kc in range(KC):
            v = p.mm(ps[:, bh, 2:4], wc[:, sl, kc, CH, :], hT[:, kc, 126:128], kc == 0, kc == KC - 1,
                     sem=s_mm if kc == KC - 1 else None)
        p.wait("scalar", s_mm, v)
        va = p.copy("scalar", chh[:, sl, :], ps[:, bh, 2:4], sem=s_ea)
        p.wait("vector", s_ea, va)
        p.seq("vector")
        ve = p.stt(u_sb[:, sl, 0:2], ps[:, bh, 0:2], flg[:, 1:2], chh[:, sl, :], ALU.mult, ALU.mult, sem=s_ev)
        ringH.release(bh, s_ev, ve)
        for r in range(2):
            a, b = 128 + r * 512, 128 + (r + 1) * 512
            bcc, bch, bcb = ring3.take(), ring3.take(), ring3.take()
            for kc in range(KC):
                p.mm(bank(bcc), wc[:, sl, kc, CCI, :], hT[:, kc, a:b], kc == 0, kc == KC - 1)
            for kc in range(KC):
                vch = p.mm(bank(bch), wc[:, sl, kc, CH, :], hT[:, kc, a:b], kc == 0, kc == KC - 1,
                           sem=s_mm if kc == KC - 1 else None)
            for kc in range(KC):
                vcb = p.mm(bank(bcb), wc[:, sl, kc, CB, :], hT[:, kc, a:b], kc == 0, kc == KC - 1,
                           sem=s_mm if kc == KC - 1 else None)
            cs_ = k_ % 2
            k_ += 1
            p.wait("scalar", s_mm, vch)
            p.wait("scalar", s_ev, ve)
            va = p.copy("scalar", chs[:, cs_, :], bank(bch), sem=s_ea)
            ring3.release(bch, s_ea, va)
            p.wait("vector", s_ea, va)
            p.seq("vector")
            uo = 2 + r * 512
            ve = p.tt(u_sb[:, sl, uo:uo + 512], bank(bcc), chs[:, cs_, :], ALU.mult, sem=s_ev)
            ring3.release(bcc, s_ev, ve)
            p.seq("vector")
            p.ts(ybuf[:, cs_, :], u_sb[:, sl, uo - 2:uo + 510], cw[:, j, 0:1], None, ALU.mult, sem=s_ev)
            p.seq("vector")
            p.stt(ybuf[:, cs_, :], u_sb[:, sl, uo - 1:uo + 511], cw[:, j, 1:2], ybuf[:, cs_, :], ALU.mult, ALU.add, sem=s_ev)
            p.seq("vector")
            p.stt(ybuf[:, cs_, :], u_sb[:, sl, uo:uo + 512], cw[:, j, 2:3], ybuf[:, cs_, :], ALU.mult, ALU.add, sem=s_ev)
            p.seq("vector")
            p.wait("vector", s_mm, vcb)
            ve = p.tt(catT[:, 8 + j, r * 512:(r + 1) * 512], bank(bcb), ybuf[:, cs_, :], ALU.mult, sem=s_ev)
            ring3.release(bcb, s_ev, ve)
        wcfree[j] = vcb
        if j + 2 < 8:
            issue_wc(j + 2)
    p.emit()
    for g in (g_y, g_chh, g_chs, g_u, g_cw, g_wc):
        free(g)
    free(g_h)
    if upto == 3:
        return nc

    p = Prog(nc, es, "p3")
    g_ab, ab = sb("ab", [128, 4, 2, 512], F32)
    g_es, esink = sb("esink", [128, 8], F32)
    g_tmp, tmp = sb("tmp", [128, 2, 2, 512], F32)
    g_pt, PT = sb("PT", [128, 2, 2, 512], BF16)
    g_dt, dtmp = sb("dtmp", [128, 2, 256], F32)
    s_ld = p.sem("ld")
    s_s = p.sem("s")
    s_ev = p.sem("ev")
    s_ea = p.sem("ea")
    s_o = p.sem("o")
    s_n = p.sem("n")
    p.dma("sync", ab[:], abias[:, :, :, :], s_ld)
    p.dma("sync", esink[:], sinks[:, :], s_ld)
    p.wait("scalar", s_ld)
    v_es = p.act(esink[:], esink[:], AF.Exp, sem=s_ea)
    p.wait("vector", s_ld)
    p.wait("vector", s_ea, v_es)
    ringS = BankRing(p, [0, 1, 2, 3])
    ringO = BankRing(p, [4, 5])
    pt_free = {}
    act_done3 = {}
    exp_done = {}

    def stage_a(un, qb, g):
        up = un % 2
        sbanks = [ringS.take(), ringS.take()]
        for par in range(2):
            po = 64 * par
            for kb in range(2):
                kt = qb + kb
                for hh in range(2):
                    c = 2 * g + hh
                    col = (kb * 2 + hh) * 128
                    v = p.mm(ps[:, sbanks[par], col:col + 128], kT[po:po + 64, g, kt * 128:(kt + 1) * 128],
                             qT[po:po + 64, c, qb * 128:(qb + 1) * 128], True, True,
                             sem=s_s if (kb == 1 and hh == 1) else None)
            p.wait("vector", s_s, v)
            if un >= 2:
                p.wait("vector", s_ea, act_done3[(un - 2, par)])
            ve = p.stt(tmp[:, up, par, :], bank(sbanks[par]), 0.125, ab[:, g, par, :], ALU.mult, ALU.add, sem=s_ev)
            ringS.release(sbanks[par], s_ev, ve)
            p.wait("scalar", s_ev, ve)
            if un >= 2:
                p.wait("scalar", s_o, pt_free[un - 2])
            if qb == 0:
                p.act(PT[:, up, par, 0:256], tmp[:, up, par, 0:256], AF.Exp, sem=s_ea, bias=flg[:, 0:1])
                va = p.act(PT[:, up, par, 256:512], tmp[:, up, par, 256:512], AF.Exp, sem=s_ea)
            else:
                va = p.act(PT[:, up, par, :], tmp[:, up, par, :], AF.Exp, sem=s_ea)
            act_done3[(un, par)] = va
        exp_done[un] = va

    def stage_b(un, qb, g):
        up = un % 2
        p.wait("tensor", s_ea, exp_done[un])
        bo = ringO.take()
        for which in range(2):
            for hh in range(2):
                col = which * 256 + hh * 128
                for par in range(2):
                    for kb in range(2):
                        kt = qb + kb
                        lhs = V[:, kt, g * 64:(g + 1) * 64] if which == 0 else onesb[:, :]
                        pc = (kb * 2 + hh) * 128
                        last = (which == 1 and hh == 1 and par == 1 and kb == 1)
                        v = p.mm(ps[par * 64:(par + 1) * 64, bo, col:col + 128], lhs, PT[:, up, par, pc:pc + 128],
                                 kb == 0, kb == 1, sem=s_o if last else None)
        pt_free[un] = v
        p.wait("vector", s_o, v)
        for hh in range(2):
            c = 2 * g + hh
            p.ts(dtmp[:, up, hh * 128:(hh + 1) * 128], ps[:, bo, 256 + hh * 128:256 + (hh + 1) * 128],
                 esink[:, c:c + 1], None, ALU.add, sem=s_n)
        p.seq("vector")
        p.op("vector", lambda e, up=up: e.reciprocal(out=dtmp[:, up, :], in_=dtmp[:, up, :]), s_n)
        p.seq("vector")
        for hh in range(2):
            c = 2 * g + hh
            ve = p.tt(catT[:, c, qb * 128:(qb + 1) * 128], ps[:, bo, hh * 128:(hh + 1) * 128],
                      dtmp[:, up, hh * 128:(hh + 1) * 128], ALU.mult, sem=s_n)
        ringO.release(bo, s_n, ve)

    g_wm2, wm2 = sb("wm2", [128, 2, KC, 512], BF16)
    g_mr2, modrow2 = sb("modrow2", [1, D], F32)
    g_bb, biasb = sb("biasb", [128, 2, D], F32)
    s_w2 = p.semring("w2", 2)
    s_m2 = p.sem("m2")
    s_e2 = p.sem("e2")
    s_b2 = p.sem("b2")
    s_bb = p.semring("bb", 2)
    wmv = w_mod.rearrange("(kc p) n -> p kc n", p=128)
    SEGS = [(2, g1p, 1.0, None), (3, sh2b, 0.0, 2), (4, sc2b, 1.0, 3), (5, g2p, 1.0, None)]
    sblocks = [(si, blk) for si in range(4) for blk in range(4)]
    w2full = {}
    w2free = {}
    bb_full = {}
    bb_free = {}
    sst = {"b6": None, "bc": None}

    def issue_w2(i):
        si, blk = sblocks[i]
        b = SEGS[si][0] * 4 + blk
        if i >= 2:
            p.wait("gpsimd", s_m2, w2free[i - 2])
        w2full[i] = p.dma("gpsimd", wm2[:, i % 2], wmv[:, :, b * 512:(b + 1) * 512], s_w2[i % 2])

    def issue_bb(si):
        seg = SEGS[si][0]
        if si >= 2:
            p.wait("sync", s_e2, bb_free[si - 2])
        bb_full[si] = p.dma("sync", biasb[:, si % 2, :], b_mod[0:1, seg * 2048:(seg + 1) * 2048].broadcast_to([128, D]), s_bb[si % 2])

    issue_w2(0)
    issue_w2(1)
    issue_bb(0)
    issue_bb(1)

    def block_step(i):
        si, blk = sblocks[i]
        seg, gdst, plus, colg = SEGS[si]
        p.wait("tensor", s_w2[i % 2], w2full[i])
        if sst["b6"] is not None:
            p.wait("tensor", *sst["b6"])
        for kc in range(KC):
            v = p.mm(ps[0:1, 6, :], cs_bf[:, kc:kc + 1], wm2[:, i % 2, kc, :], kc == 0, kc == KC - 1,
                     sem=s_m2 if kc == KC - 1 else None)
        v_row = v
        if colg is not None:
            if sst.get("b7") is not None:
                p.wait("tensor", *sst["b7"])
            for c4 in range(4):
                idx = (colg - 2) * 16 + blk * 4 + c4
                for kc in range(KC):
                    v = p.mm(ps[:, 7, idx:idx + 1], wm2[:, i % 2, kc, c4 * 128:(c4 + 1) * 128], cs_bf[:, kc:kc + 1],
                             kc == 0, kc == KC - 1, sem=s_m2 if (kc == KC - 1 and c4 == 3) else None)
        w2free[i] = v
        p.wait("vector", s_m2, v_row)
        if blk == 0 and sst["bc"] is not None:
            p.wait("vector", *sst["bc"])
        ve = p.ts(modrow2[0:1, blk * 512:(blk + 1) * 512], ps[0:1, 6, :], plus, None, ALU.add, sem=s_e2)
        sst["b6"] = (s_e2, ve)
        if i + 2 < len(sblocks):
            issue_w2(i + 2)

    def seg_end_step(si):
        seg, gdst, plus, colg = SEGS[si]
        p.wait("tensor", *sst["b6"])
        p.wait("vector", s_bb[si % 2], bb_full[si])
        ve = None
        for n in range(4):
            if ve is not None:
                p.wait("tensor", s_e2, ve)
            v = p.mm(bank(6), ones_row[0:1, :], modrow2[0:1, n * 512:(n + 1) * 512], True, True, sem=s_b2)
            p.wait("vector", s_b2, v)
            ve = p.tt(gdst[:, n * 512:(n + 1) * 512], bank(6), biasb[:, si % 2, n * 512:(n + 1) * 512], ALU.add, sem=s_e2)
        sst["b6"] = (s_e2, ve)
        sst["bc"] = (s_b2, v)
        bb_free[si] = ve
        if si + 2 < 4:
            issue_bb(si + 2)
        if colg is not None:
            c0 = 48 if colg == 2 else 64
            vb7 = p.tt(colv[:, colg * 16:(colg + 1) * 16], ps[:, 7, (colg - 2) * 16:(colg - 1) * 16], bmc[:, c0:c0 + 16], ALU.add, sem=s_e2)
            sst["b7"] = (s_e2, vb7)
            if colg == 3:
                p.seq("vector")
                p.ts(colv[:, 48:64], colv[:, 48:64], 1.0, None, ALU.add, sem=s_e2)

    side_steps = []
    for si in range(4):
        for blk in range(4):
            side_steps.append((block_step, si * 4 + blk))
        side_steps.append((seg_end_step, si))

    unit_list = [(qb, g) for qb in range(NT) for g in range(4)]
    stage_a(0, *unit_list[0])
    for un, (qb, g) in enumerate(unit_list):
        if un + 1 < len(unit_list):
            stage_a(un + 1, *unit_list[un + 1])
        stage_b(un, qb, g)
        if un % 2 == 1 and un >= 3 and side_steps:
            fn, arg = side_steps.pop(0)
            fn(arg)
    while side_steps:
        fn, arg = side_steps.pop(0)
        fn(arg)
    if debug:
        s_st = p.sem("st")
        p.wait("sync", s_n)
        p.dma("sync", dbg["d_cat"][:, :, :], catT[:], s_st)
        p.wait("sync", s_st)
    p.emit()
    for g in (g_bb, g_mr2, g_wm2):
        free(g)
    for g in (g_dt, g_pt, g_tmp, g_es, g_ab):
        free(g)
    for g in (g_v, g_k, g_q):
        free(g)
    if upto == 4:
        return nc

    g_R, R = sb("R", [128, NT, D], F32)
    p = Prog(nc, es, "p4a")
    g_wo, wo = sb("wo", [128, 2, KC, 512], BF16)
    g_xt, xt = sb("xt", [128, 3, 512], F32)
    s_w = p.semring("w", 2)
    s_x = p.semring("x", 3)
    s_mm = p.sem("mm")
    s_ev = p.sem("ev")
    wov = w_out.rearrange("(kc p) n -> p kc n", p=128)
    wofull = {}
    wofree = {}

    def issue_wo(n):
        if n >= 2:
            p.wait("gpsimd", s_mm, wofree[n - 2])
        wofull[n] = p.dma("gpsimd", wo[:, n % 2], wov[:, :, n * 512:(n + 1) * 512], s_w[n % 2])

    issue_wo(0)
    issue_wo(1)
    xfull = {}
    xfree = {}
    items = [(n, t) for n in range(4) for t in range(NT)]

    def issue_xt(i):
        n, t = items[i]
        if i >= 3:
            p.wait("sync", s_ev, xfree[i - 3])
        xfull[i] = p.dma("sync", xt[:, i % 3, :], xh[128 + t * 128:128 + (t + 1) * 128, n * 512:(n + 1) * 512], s_x[i % 3])

    for i in range(3):
        issue_xt(i)
    ring = BankRing(p, [0, 1, 2, 3])
    for i, (n, t) in enumerate(items):
        if t == 0:
            p.wait("tensor", s_w[n % 2], wofull[n])
        bk = ring.take()
        for kc in range(KC):
            v = p.mm(bank(bk), catT[:, kc, t * 128:(t + 1) * 128], wo[:, n % 2, kc, :], kc == 0, kc == KC - 1,
                     sem=s_mm if kc == KC - 1 else None)
        if t == NT - 1:
            wofree[n] = v
            if n + 2 < 4:
                issue_wo(n + 2)
        p.wait("vector", s_mm, v)
        dst = R[:, t, n * 512:(n + 1) * 512]
        ve = p.tt(dst, bank(bk), g1p[:, n * 512:(n + 1) * 512], ALU.mult, sem=s_ev)
        ring.release(bk, s_ev, ve)
        p.wait("vector", s_x[i % 3], xfull[i])
        p.seq("vector")
        xfree[i] = p.stt(dst, xt[:, i % 3, :], ALPHA, dst, ALU.mult, ALU.add, sem=s_ev)
        if i + 3 < len(items):
            issue_xt(i + 3)
    p.emit()
    for g in (g_xt, g_wo):
        free(g)
    free(g_cat)
    if upto == 5:
        return nc

    g_h2, h2T = sb("h2T", [128, KC, TOWN], BF16, side="right")
    p = Prog(nc, es, "p4b")
    g_lg, lng = sb("lng", [128, D], F32)
    g_lb, lnb = sb("lnb", [128, D], F32)
    g_wr, wr = sb("wr", [128, KC, NE], F32)
    g_rb, rbias = sb("rbias", [128, NE], F32)
    g_hf, h2f = sb("h2f", [128, 2, KC, 128], F32)
    g_st, stats = sb("stats", [128, 4, 6], F32)
    g_mv, mv = sb("mv", [128, 2], F32)
    g_rs, rstd = sb("rstd", [128, 1], F32)
    g_gt, gt = sb("gt", [128, 16, NE], F32)
    g_g8, g8 = sb("g8", [128, 10, 8], F32)
    g_hk, h2tok = sb("h2tok", [128, 3, D], BF16)
    g_ht, h2tmp = sb("h2tmp", [128, D], F32)
    g_cb, chb = sb("chb", [128, NT, NE], BF16)
    g_lt, ltri = sb("ltri_sb", [128, 128], BF16)
    g_o1, ones128 = sb("ones128", [128, 128], BF16)
    g_eb, ebase = sb("ebase_sb", [128, NE], F32)
    s_ld = p.sem("ld")
    s_v = p.sem("v")
    s_a = p.sem("a")
    s_t = p.sem("t")
    s_r = p.sem("r")
    s_st = p.sem("st")
    s_pl = p.sem("pl")
    s_sc = p.semring("sc", 3)
    s_ps = p.sem("ps")
    p.dma("sync", lng[:], ln1_g.broadcast_to([128, D]), s_ld)
    p.dma("sync", lnb[:], ln1_b.broadcast_to([128, D]), s_ld)
    p.dma("sync", wr[:], w_router.rearrange("(kc p) n -> p kc n", p=128), s_ld)
    p.dma("sync", rbias[:], router_bias.broadcast_to([128, NE]), s_ld)
    p.dma("sync", ebase[:], ebase_d[:, :], s_ld)
    s_lt = p.sem("lt")
    p.dma("gpsimd", ltri[:], ltri_d[:, :], s_lt)
    p.op("vector", lambda e: e.memset(ones128[:], 1.0))
    v_o128 = p.last["vector"]
    p.wait("vector", s_ld)
    p.wait("gpsimd", s_ld)
    p.wait("tensor", s_ld)
    p.wait("tensor", s_lt)
    p.wait("tensor", *v_o128)
    ringT = BankRing(p, [0, 1, 2, 3, 4, 5])
    ringL = BankRing(p, [6, 7])
    hf_free = {}
    sc_done = {}
    dest_ready = {}
    regs = {}

    def _mkreg(e):
        regs["b"] = e.alloc_register("bnd4b")
        e.reg_mov(regs["b"], NE * CAP - 1)

    p.raw("gpsimd", _mkreg)

    h2_ready = {}

    def scatter_tile(t):
        p.wait("gpsimd", s_v, h2_ready[t])
        for k in range(8):
            sc_done[t] = p.op("gpsimd", lambda e, t=t, k=k: e.indirect_dma_start(
                out=hd[:, :], out_offset=bass.IndirectOffsetOnAxis(ap=dest_i[:, t, k:k + 1].bitcast(U32), axis=0),
                in_=h2tok[:, t % 3, :], in_offset=None, bounds_check=regs["b"], oob_is_err=False), s_sc[t % 3], dma=True)

    x1_done = {0: layer_norm_tile(p, R[:, 0, :], lng, lnb, stats, mv, rstd, s_v, s_a, s_pl)}
    for t in range(NT):
        v_x1 = x1_done[t]
        if t + 1 < NT:
            x1_done[t + 1] = layer_norm_tile(p, R[:, t + 1, :], lng, lnb, stats, mv, rstd, s_v, s_a, s_pl)
        p.wait("sync", *v_x1)
        p.dma("sync", x1s[t * 128:(t + 1) * 128, :], R[:, t, :], s_st)
        if debug:
            p.dma("sync", dbg["d_x1"][t * 128:(t + 1) * 128, :], R[:, t, :], s_st)
        p.wait("tensor", *v_x1)
        hs = t % 2
        if t >= 2:
            p.wait("scalar", s_r, hf_free[t - 2][0])
            p.wait("scalar", s_v, hf_free[t - 2][1])
        for q4 in range(4):
            bk = ringT.take()
            for i in range(4):
                kc = q4 * 4 + i
                v = p.tr(ps[:, bk, i * 128:(i + 1) * 128], R[:, t, kc * 128:(kc + 1) * 128], ident[:],
                         sem=s_t if i == 3 else None)
            p.wait("scalar", s_t, v)
            for i in range(4):
                kc = q4 * 4 + i
                va = p.act(h2f[:, hs, kc, :], ps[:, bk, i * 128:(i + 1) * 128], AF.Identity, sem=s_a,
                           scale=colv[:, SC2 + kc:SC2 + kc + 1], bias=colv[:, SH2 + kc:SH2 + kc + 1])
            ringT.release(bk, s_a, va)
        p.wait("vector", s_a, va)
        v_cast = p.copy("vector", h2T[:, :, t * 128:(t + 1) * 128], h2f[:, hs, :, :], sem=s_v)
        p.wait("tensor", s_a, va)
        bl = ringL.take()
        for kc in range(KC):
            v = p.mm(ps[:, bl, 0:NE], h2f[:, hs, kc, :], wr[:, kc, :], kc == 0, kc == KC - 1,
                     sem=s_r if kc == KC - 1 else None)
        hf_free[t] = (v, v_cast)
        vg = gating_tile(p, ps[:, bl, 0:NE], rbias, gt, g8, G[:, t, 0:NE], s_r, v, s_v, s_a)
        ringL.release(bl, s_a, vg)
        chosen = gt[:, 5, :]
        dd, v1_, valid, aa, negd, Gv, oh = (gt[:, 8 + i, :] for i in range(7))
        top8 = g8[:, 8, :]

        def V_(fn):
            p.seq("vector")
            return p.op("vector", fn, s_v)

        v_ch = V_(lambda e, t=t: e.tensor_copy(out=chb[:, t, :], in_=chosen))
        p.wait("tensor", s_v, v_ch)
        bp = ringL.take()
        for tp in range(t):
            p.mm(ps[:, bp, 0:NE], ones128[:], chb[:, tp, :], tp == 0, False)
        v_pos = p.mm(ps[:, bp, 0:NE], ltri[:], chb[:, t, :], t == 0, True, sem=s_ps)
        p.wait("vector", s_ps, v_pos)
        pos_ps = ps[:, bp, 0:NE]
        V_(lambda e: e.tensor_tensor(out=dd, in0=pos_ps, in1=ebase[:], op=ALU.add))
        v_rel = V_(lambda e: e.tensor_scalar(out=v1_, in0=pos_ps, scalar1=CAP - 0.5, scalar2=None, op0=ALU.is_lt))
        ringL.release(bp, s_v, v_rel)
        V_(lambda e: e.tensor_tensor(out=valid, in0=v1_, in1=chosen, op=ALU.mult))
        V_(lambda e: e.tensor_scalar(out=aa, in0=dd, scalar1=-1.0, scalar2=BIG, op0=ALU.mult, op1=ALU.add))
        V_(lambda e: e.tensor_tensor(out=aa, in0=aa, in1=valid, op=ALU.mult))
        V_(lambda e: e.tensor_scalar(out=negd, in0=aa, scalar1=-BIG, scalar2=None, op0=ALU.add))
        V_(lambda e: e.max(out=top8, in_=negd))
        dest_ready[t] = V_(lambda e, t=t: e.tensor_scalar(out=dest_i[:, t, :], in0=top8, scalar1=-1.0, scalar2=None, op0=ALU.mult))
        V_(lambda e, t=t: e.tensor_tensor(out=Gv, in0=G[:, t, 0:NE], in1=valid, op=ALU.mult))
        for k in range(8):
            V_(lambda e, k=k: e.scalar_tensor_tensor(out=oh, in0=negd, scalar=top8[:, k:k + 1], in1=Gv, op0=ALU.is_equal, op1=ALU.mult))
            V_(lambda e, t=t, k=k: e.tensor_reduce(out=wk[:, t, k:k + 1], in_=oh, axis=AX.X, op=ALU.add))
        p.wait("vector", *v_x1)
        if t >= 3:
            p.wait("vector", s_sc[t % 3], sc_done[t - 3])
        V_(lambda e, t=t: e.tensor_tensor(out=h2tmp[:], in0=R[:, t, :], in1=sc2b[:], op=ALU.mult))
        h2_ready[t] = V_(lambda e, t=t: e.tensor_tensor(out=h2tok[:, t % 3, :], in0=h2tmp[:], in1=sh2b[:], op=ALU.add))
        scatter_tile(t)
    g_pc, pcol = sb("pcol_sb", [128, NS], F32)
    g_ix, idxf = sb("idxf", [128, 3, NE], F32)
    s_pc = p.sem("pc")
    v_pc = p.dma("sync", pcol[:], pcol_d[:, :], s_pc)
    bc_ = ringL.take()
    for tp in range(NT):
        v_cnt = p.mm(ps[:, bc_, 0:NE], ones128[:], chb[:, tp, :], tp == 0, tp == NT - 1, sem=s_ps if tp == NT - 1 else None)
    p.wait("vector", s_ps, v_cnt)
    p.wait("vector", s_pc, v_pc)
    for st_ in range(NS):
        p.seq("vector")
        p.ts(idxf[:, 0, :], ps[:, bc_, 0:NE], pcol[:, st_:st_ + 1], None, ALU.is_gt, sem=s_v)
        p.ts(idxf[:, 1, :], ebase[:], pcol[:, st_:st_ + 1], -BIG, ALU.add, ALU.add, sem=s_v)
        p.seq("vector")
        p.tt(idxf[:, 2, :], idxf[:, 0, :], idxf[:, 1, :], ALU.mult, sem=s_v)
        p.seq("vector")
        p.ts(idx_i[:, st_, :], idxf[:, 2, :], BIG, None, ALU.add, sem=s_v)
    p.wait("sync", s_st)
    for q_ in s_sc:
        p.wait("gpsimd", q_)
    p.raw("gpsimd", lambda e: e.free_register(regs["b"]))
    if debug:
        p.seq("vector")
        p.wait("sync", *p.last["vector"])
        p.dma("sync", dbg["d_G"][:, :, :], G[:], s_st)
        p.dma("sync", dbg["d_dest"][:, :, :], dest_i[:], s_st)
        p.dma("sync", dbg["d_wk"][:, :, :], wk[:], s_st)
        p.wait("sync", s_st)
    p.emit()
    for g in (g_ix, g_pc, g_eb, g_o1, g_lt, g_cb, g_ht, g_hk, g_g8, g_gt, g_rs, g_mv, g_st, g_hf, g_rb, g_wr, g_lb, g_lg):
        free(g)
    free(g_R)
    if upto == 6:
        return nc

    p = Prog(nc, es, "p5a")
    g_gu, gu = sb("gu", [128, 3, 2, KC, 128], BF16)
    g_wd, wd = sb("wd", [128, 2, 4, D], BF16)
    g_sg, sg = sb("sg", [128, 2, 512], F32)
    g_at, actT = sb("actT", [128, 2, 4, TOWN], BF16)
    g_yb, ybuf = sb("ybuf", [128, 2, D], F32)
    s_gu = [p.sem("gu%d" % i) for i in range(3)]
    s_wd = p.sem("wd")
    s_mm = p.sem("mm")
    s_dn = p.sem("dn")
    s_a = p.sem("a")
    s_v = p.sem("v")
    s_y = p.semring("y", 2)
    gufull = {}
    gufree = {}

    def issue_gus(j):
        if j >= 3:
            p.wait("gpsimd", s_mm, gufree[j - 3])
        p.dma("gpsimd", gu[:, j % 3, 0], ws_gate.rearrange("(kc p) (j i) -> p kc j i", p=128, j=4)[:, :, j, :], s_gu[j % 3])
        gufull[j] = p.dma("gpsimd", gu[:, j % 3, 1], ws_up.rearrange("(kc p) (j i) -> p kc j i", p=128, j=4)[:, :, j, :], s_gu[j % 3])

    for j in range(3):
        issue_gus(j)
    v_wd = p.dma("gpsimd", wd[:, 0], ws_down.rearrange("(j p) n -> p j n", p=128), s_wd)
    ringGU = BankRing(p, [0, 1, 2, 3])
    ringD = BankRing(p, [4, 5, 6, 7])
    sg_k = 0
    sg_free = {}
    for j in range(4):
        p.wait("tensor", s_gu[j % 3], gufull[j])
        for th in range(2):
            bg, bu = ringGU.take(), ringGU.take()
            for kc in range(KC):
                vg_ = p.mm(bank(bg), gu[:, j % 3, 0, kc, :], h2T[:, kc, th * 512:(th + 1) * 512], kc == 0, kc == KC - 1,
                           sem=s_mm if kc == KC - 1 else None)
            for kc in range(KC):
                vu_ = p.mm(bank(bu), gu[:, j % 3, 1, kc, :], h2T[:, kc, th * 512:(th + 1) * 512], kc == 0, kc == KC - 1,
                           sem=s_mm if kc == KC - 1 else None)
            sk = sg_k % 2
            p.wait("scalar", s_mm, vg_)
            if sg_k >= 2:
                p.wait("scalar", s_v, sg_free[sg_k - 2])
            va = p.act(sg[:, sk, :], bank(bg), AF.Silu, sem=s_a)
            ringGU.release(bg, s_a, va)
            p.wait("vector", s_a, va)
            p.wait("vector", s_mm, vu_)
            vv = p.tt(actT[:, 0, j, th * 512:(th + 1) * 512], bank(bu), sg[:, sk, :], ALU.mult, sem=s_v)
            ringGU.release(bu, s_v, vv)
            sg_free[sg_k] = vv
            sg_k += 1
        gufree[j] = vu_
        if j + 3 < 4:
            issue_gus(j + 3)
    p.wait("tensor", s_wd, v_wd)
    p.wait("tensor", s_v, vv)
    ydma = {}
    for t in range(NT):
        yb = t % 2
        if t >= 2:
            p.wait("scalar", s_y[yb], ydma[t - 2])
        for n in range(4):
            bk = ringD.take()
            for j in range(4):
                v = p.mm(bank(bk), actT[:, 0, j, t * 128:(t + 1) * 128], wd[:, 0, j, n * 512:(n + 1) * 512],
                         j == 0, j == 3, sem=s_dn if j == 3 else None)
            p.wait("scalar", s_dn, v)
            va = p.act(ybuf[:, yb, n * 512:(n + 1) * 512], bank(bk), AF.Identity, sem=s_a)
            ringD.release(bk, s_a, va)
        p.wait("sync", s_a, va)
        ydma[t] = p.dma("sync", ysh[t * 128:(t + 1) * 128, :], ybuf[:, yb, :], s_y[yb])
    for q_ in s_y:
        p.wait("sync", q_)
    p.emit()
    for g in (g_yb, g_at, g_sg, g_wd, g_gu):
        free(g)
    free(g_h2)
    if upto == 7:
        return nc

    p = Prog(nc, es, "p5b")
    g_gu, gu = sb("gu", [128, 3, 2, KC, 256], BF16)
    g_wd, wd = sb("wd", [128, 2, 4, D], BF16)
    g_sg, sg = sb("sg", [128, 2, CAP], F32)
    g_at, actT = sb("actT", [128, 2, 4, CAP], BF16)
    g_yb, ybuf = sb("ybuf", [128, 3, D], F32)
    NSLOT = 4
    g_hs, hsel = sb("hsel", [128, NSLOT, D], BF16)
    g_hT, hselT = sb("hselT", [128, 2, KC, CAP], BF16)
    g_ib, identb = sb("identb", [128, 128], BF16)
    s_gu = [p.sem("gu%d" % i) for i in range(3)]
    s_hs = [p.sem("hs%d" % i) for i in range(NSLOT)]
    s_wd = p.semring("wd", 2)
    s_mm = p.sem("mm")
    s_dn = p.sem("dn")
    s_a = p.sem("a")
    s_v = p.sem("v")
    s_y = p.semring("y", 3)
    s_t = p.sem("t")
    s_ta = p.sem("ta")
    s_tv = p.sem("tv")
    s_ya = p.sem("ya")
    v_ib = p.copy("vector", identb[:], ident[:], sem=s_v)
    p.wait("tensor", s_v, v_ib)
    units = [(e, jp) for e in range(NE) for jp in range(2)]
    gufull = {}
    gufree = {}
    wdfull = {}
    wdfree = {}
    hsfull = {}
    hsfree = {}

    def issue_gu(i):
        e, jp = units[i]
        if i >= 3:
            p.wait("gpsimd", s_mm, gufree[i - 3])
        p.dma("gpsimd", gu[:, i % 3, 0], w_gate[e].rearrange("(kc p) (j i) -> p kc j i", p=128, j=2)[:, :, jp, :], s_gu[i % 3])
        gufull[i] = p.dma("gpsimd", gu[:, i % 3, 1], w_up[e].rearrange("(kc p) (j i) -> p kc j i", p=128, j=2)[:, :, jp, :], s_gu[i % 3])

    def issue_wd(e):
        if e >= 2:
            p.wait("gpsimd", s_dn, wdfree[e - 2])
        wdfull[e] = p.dma("gpsimd", wd[:, e % 2], w_down[e].rearrange("(j p) n -> p j n", p=128), s_wd[e % 2])

    regs5 = {}

    def _mkreg5(e):
        regs5["b"] = e.alloc_register("bnd5b")
        e.reg_mov(regs5["b"], NE * CAP - 1)

    p.raw("gpsimd", _mkreg5)
    p.op("vector", lambda e: e.memset(hsel[:, 0:2, :], 0.0))
    p.op("vector", lambda e: e.memset(hsel[:, 2:4, :], 0.0))
    p.wait("gpsimd", *p.last["vector"])

    def issue_hs(i):
        e_, st_ = divmod(i, NS)
        if i >= NSLOT:
            p.wait("gpsimd", s_t, hsfree[i - NSLOT])
        hsfull[i] = p.op("gpsimd", lambda e, e_=e_, st_=st_, i=i: e.indirect_dma_start(
            out=hsel[:, i % NSLOT, :], out_offset=None, in_=hd[:, :],
            in_offset=bass.IndirectOffsetOnAxis(ap=idx_i[:, st_, e_:e_ + 1].bitcast(U32), axis=0),
            bounds_check=regs5["b"], oob_is_err=False), s_hs[i % NSLOT], dma=True)

    for i in range(3):
        issue_gu(i)
    issue_wd(0)
    issue_wd(1)
    for i in range(NSLOT):
        issue_hs(i)
    ringGU = BankRing(p, [0, 1, 2, 3])
    ringD = BankRing(p, [4, 5])
    ringTr = BankRing(p, [6, 7])
    T_done = {}
    gu_done = {}
    act_done = {}
    d_done = {}
    ydma = {}
    ycnt = [0]
    sgk = [0]
    sg_free = {}

    def emit_T(e):
        eb = e % 2
        for st in range(NS):
            i = e * NS + st
            slot = i % NSLOT
            p.wait("tensor", s_hs[slot], hsfull[i])
            for half in range(2):
                bk = ringTr.take()
                psb = ps[:, bk, :].bitcast(BF16)
                for q in range(8):
                    kc = half * 8 + q
                    v = p.tr(psb[:, q * 128:(q + 1) * 128], hsel[:, slot, kc * 128:(kc + 1) * 128], identb[:],
                             sem=s_t if q == 7 else None)
                eng = "scalar" if half == 0 else "vector"
                sem = s_ta if half == 0 else s_tv
                p.wait(eng, s_t, v)
                if e >= 2 and st == 0:
                    p.wait(eng, s_mm, gu_done[e - 2])
                ve = p.copy(eng, hselT[:, eb, half * 8:(half + 1) * 8, st * 128:(st + 1) * 128],
                            psb[:, :].rearrange("p (q s) -> p q s", q=8), sem=sem)
                ringTr.release(bk, sem, ve)
                if half == 0:
                    va_ = ve
                else:
                    vv_ = ve
            hsfree[i] = v
            if i + NSLOT < NE * NS:
                issue_hs(i + NSLOT)
        T_done[e] = (va_, vv_)

    def emit_GU(e):
        eb = e % 2
        p.wait("tensor", s_ta, T_done[e][0])
        p.wait("tensor", s_tv, T_done[e][1])
        for j in range(4):
            i = e * 2 + j // 2
            jo = (j % 2) * 128
            if j % 2 == 0:
                p.wait("tensor", s_gu[i % 3], gufull[i])
            bg, bu = ringGU.take(), ringGU.take()
            for kc in range(KC):
                vg_ = p.mm(ps[:, bg, 0:CAP], gu[:, i % 3, 0, kc, jo:jo + 128], hselT[:, eb, kc, :], kc == 0, kc == KC - 1,
                           sem=s_mm if kc == KC - 1 else None)
            for kc in range(KC):
                vu_ = p.mm(ps[:, bu, 0:CAP], gu[:, i % 3, 1, kc, jo:jo + 128], hselT[:, eb, kc, :], kc == 0, kc == KC - 1,
                           sem=s_mm if kc == KC - 1 else None)
            sk = sgk[0] % 2
            p.wait("scalar", s_mm, vg_)
            if sgk[0] >= 2:
                p.wait("scalar", s_v, sg_free[sgk[0] - 2])
            va = p.act(sg[:, sk, :], ps[:, bg, 0:CAP], AF.Silu, sem=s_a)
            ringGU.release(bg, s_a, va)
            p.wait("vector", s_a, va)
            p.wait("vector", s_mm, vu_)
            if j == 0 and e >= 2:
                p.wait("vector", s_dn, d_done[e - 2])
            vv = p.tt(actT[:, eb, j, :], ps[:, bu, 0:CAP], sg[:, sk, :], ALU.mult, sem=s_v)
            ringGU.release(bu, s_v, vv)
            sg_free[sgk[0]] = vv
            sgk[0] += 1
            if j % 2 == 1:
                gufree[i] = vu_
                if i + 3 < len(units):
                    issue_gu(i + 3)
        gu_done[e] = vu_
        act_done[e] = vv

    def emit_D(e):
        eb = e % 2
        p.wait("tensor", s_wd[e % 2], wdfull[e])
        p.wait("tensor", s_v, act_done[e])
        for st in range(NS):
            u = ycnt[0]
            ycnt[0] += 1
            yb = u % 3
            if u >= 3:
                p.wait("scalar", s_y[yb], ydma[u - 3])
            for n in range(4):
                bk = ringD.take()
                for j in range(4):
                    v = p.mm(bank(bk), actT[:, eb, j, st * 128:(st + 1) * 128], wd[:, e % 2, j, n * 512:(n + 1) * 512],
                             j == 0, j == 3, sem=s_dn if j == 3 else None)
                p.wait("scalar", s_dn, v)
                va = p.act(ybuf[:, yb, n * 512:(n + 1) * 512], bank(bk), AF.Identity, sem=s_ya)
                ringD.release(bk, s_ya, va)
            p.wait("gpsimd", s_ya, va)
            ydma[u] = p.op("gpsimd", lambda e_, ex=e, st=st, yb=yb: e_.indirect_dma_start(
                out=yd[:, :], out_offset=bass.IndirectOffsetOnAxis(ap=idx_i[:, st, ex:ex + 1].bitcast(U32), axis=0),
                in_=ybuf[:, yb, :], in_offset=None, bounds_check=regs5["b"], oob_is_err=False), s_y[yb], dma=True)
        d_done[e] = v
        wdfree[e] = v
        if e + 2 < NE:
            issue_wd(e + 2)

    emit_T(0)
    for e in range(NE):
        emit_GU(e)
        if e + 1 < NE:
            emit_T(e + 1)
        emit_D(e)
    for q_ in s_y:
        p.wait("gpsimd", q_)
    p.raw("gpsimd", lambda e: e.free_register(regs5["b"]))
    p.emit()
    for g in (g_ib, g_hT, g_hs, g_yb, g_at, g_sg, g_wd, g_gu):
        free(g)
    if upto == 8:
        return nc

    p = Prog(nc, es, "p6")
    g_lg, lng = sb("lng2", [128, D], F32)
    g_lb, lnb = sb("lnb2", [128, D], F32)
    g_x1, x1t = sb("x1t", [128, 2, D], F32)
    g_ac, acct = sb("acct", [128, 2, D], F32)
    g_gb, gbuf = sb("gbuf", [128, 4, D], F32)
    g_st, stats = sb("stats2", [128, 4, 6], F32)
    g_mv, mv = sb("mv2", [128, 2], F32)
    g_rs, rstd = sb("rstd2", [128, 1], F32)
    s_ld = p.sem("ld")
    s_x = p.semring("x", 2)
    s_v = p.sem("v")
    s_a = p.sem("a")
    s_pl = p.sem("pl")
    s_st = p.semring("st", 2)
    s_g = p.semring("g", 4)
    p.dma("sync", lng[:], ln2_g.broadcast_to([128, D]), s_ld)
    p.dma("sync", lnb[:], ln2_b.broadcast_to([128, D]), s_ld)
    for i in range(4):
        p.op("vector", lambda e, i=i: e.memset(gbuf[:, i, :], 0.0))
    v_ms = p.last["vector"]
    p.wait("gpsimd", *v_ms)
    xfull = {}
    ydone = {}

    def issue_ld(t):
        if t >= 2:
            p.wait("sync", s_st[t % 2], ydone[t - 2])
        p.dma("sync", acct[:, t % 2, :], ysh[t * 128:(t + 1) * 128, :], s_x[t % 2])
        xfull[t] = p.dma("sync", x1t[:, t % 2, :], x1s[t * 128:(t + 1) * 128, :], s_x[t % 2])

    issue_ld(0)
    issue_ld(1)
    p.wait("vector", s_ld)
    gcnt = 0
    gfree = {}
    regs6 = {}

    def _mkreg6(e):
        regs6["b"] = e.alloc_register("bnd6")
        e.reg_mov(regs6["b"], NE * CAP - 1)

    p.raw("gpsimd", _mkreg6)
    for t in range(NT):
        a = t % 2
        acc = acct[:, a, :]
        p.wait("vector", s_x[t % 2], xfull[t])
        for k in range(8):
            gi = gcnt % 4
            if gcnt >= 4:
                p.wait("gpsimd", s_v, gfree[gcnt - 4])
            vg = p.op("gpsimd", lambda e, t=t, k=k, gi=gi: e.indirect_dma_start(
                out=gbuf[:, gi, :], out_offset=None, in_=yd[:, :],
                in_offset=bass.IndirectOffsetOnAxis(ap=dest_i[:, t, k:k + 1].bitcast(U32), axis=0),
                bounds_check=regs6["b"], oob_is_err=False), s_g[gi], dma=True)
            p.wait("vector", s_g[gi], vg)
            p.seq("vector")
            gfree[gcnt] = p.stt(acc, gbuf[:, gi, :], wk[:, t, k:k + 1], acc, ALU.mult, ALU.add, sem=s_v)
            gcnt += 1
        p.seq("vector")
        p.tt(acc, acc, g2p[:], ALU.mult, sem=s_v)
        p.seq("vector")
        p.stt(acc, x1t[:, a, :], ALPHA, acc, ALU.mult, ALU.add, sem=s_v)
        v_ln = layer_norm_tile(p, acc, lng, lnb, stats, mv, rstd, s_v, s_a, s_pl, gb_eng="vector")
        p.wait("sync", *v_ln)
        ydone[t] = p.dma("sync", y[t * 128:(t + 1) * 128, :], acc, s_st[t % 2])
        if t + 2 < NT:
            issue_ld(t + 2)
    for q_ in s_st:
        p.wait("sync", q_)
    p.emit()
    es.close()
    return nc


def layer_norm_tile(p, x, lng, lnb, stats, mv, rstd, s_v, s_a, s_pl, gb_eng="gpsimd"):
    p.seq("vector")
    for c in range(4):
        p.op("vector", lambda e, c=c: e.bn_stats(out=stats[:, c, :], in_=x[:, c * 512:(c + 1) * 512]), s_v)
    p.seq("vector")
    p.op("vector", lambda e: e.bn_aggr(out=mv[:], in_=stats[:].rearrange("p a b -> p (a b)")), s_v)
    p.seq("vector")
    v = p.ts(rstd[:], mv[:, 1:2], LN_EPS, None, ALU.add, sem=s_v)
    p.wait("scalar", s_v, v)
    va = p.act(rstd[:], rstd[:], AF.Sqrt, sem=s_a)
    p.wait("vector", s_a, va)
    p.op("vector", lambda e: e.reciprocal(out=rstd[:], in_=rstd[:]), s_v)
    p.seq("vector")
    vv = p.ts(x, x, mv[:, 0:1], rstd[:, 0:1], ALU.subtract, ALU.mult, sem=s_v)
    if gb_eng == "gpsimd":
        p.wait("gpsimd", s_v, vv)
        p.tt(x, x, lng[:], ALU.mult, sem=s_pl, eng="gpsimd")
        p.seq("gpsimd")
        v = p.tt(x, x, lnb[:], ALU.add, sem=s_pl, eng="gpsimd")
        return (s_pl, v)
    p.seq("vector")
    p.tt(x, x, lng[:], ALU.mult, sem=s_v)
    p.seq("vector")
    v = p.tt(x, x, lnb[:], ALU.add, sem=s_v)
    return (s_v, v)


def gating_tile(p, lg_ps, rbias, gt, g8, Gout, s_r, v_r, s_v, s_a):
    sc, sel, eq, sel2, selm, chosen, gw = (gt[:, i, :] for i in range(7))
    m1, m2, gs, rank, gmask, pen = (g8[:, i, :] for i in range(6))
    cmp = gt[:, 7, :]
    top8 = g8[:, 6, :]
    ssum = g8[:, 7, 0:1]
    rs = g8[:, 7, 1:2]

    def v3(ap):
        return ap.rearrange("p (g k) -> p g k", k=8)

    p.wait("scalar", s_r, v_r)
    p.seq("vector")
    p.wait("scalar", *p.last["vector"])
    va = p.act(sc, lg_ps, AF.Sigmoid, sem=s_a)
    p.wait("vector", s_a, va)

    def V_(fn):
        p.seq("vector")
        return p.op("vector", fn, s_v)

    V_(lambda e: e.tensor_tensor(out=sel, in0=sc, in1=rbias[:], op=ALU.add))
    V_(lambda e: e.tensor_reduce(out=m1, in_=v3(sel), axis=AX.X, op=ALU.max))
    V_(lambda e: e.tensor_tensor(out=v3(eq), in0=v3(sel), in1=m1.unsqueeze(2).broadcast_to([128, 8, 8]), op=ALU.is_equal))
    V_(lambda e: e.scalar_tensor_tensor(out=sel2, in0=eq, scalar=-1e9, in1=sel, op0=ALU.mult, op1=ALU.add))
    V_(lambda e: e.tensor_reduce(out=m2, in_=v3(sel2), axis=AX.X, op=ALU.max))
    V_(lambda e: e.tensor_tensor(out=gs, in0=m1, in1=m2, op=ALU.add))
    V_(lambda e: e.tensor_tensor(out=v3(cmp), in0=gs.unsqueeze(1).broadcast_to([128, 8, 8]),
                                 in1=gs.unsqueeze(2).broadcast_to([128, 8, 8]), op=ALU.is_gt))
    V_(lambda e: e.tensor_reduce(out=rank, in_=v3(cmp), axis=AX.X, op=ALU.add))
    V_(lambda e: e.tensor_scalar(out=gmask, in0=rank, scalar1=3.5, scalar2=None, op0=ALU.is_lt))
    V_(lambda e: e.tensor_scalar(out=pen, in0=gmask, scalar1=-1.0, scalar2=1e9, op0=ALU.add, op1=ALU.mult))
    V_(lambda e: e.tensor_tensor(out=v3(selm), in0=v3(sel), in1=pen.unsqueeze(2).broadcast_to([128, 8, 8]), op=ALU.add))
    V_(lambda e: e.max(out=top8, in_=selm))
    V_(lambda e: e.tensor_scalar(out=chosen, in0=selm, scalar1=top8[:, 7:8], scalar2=None, op0=ALU.is_ge))
    V_(lambda e: e.tensor_tensor(out=gw, in0=chosen, in1=sc, op=ALU.mult))
    V_(lambda e: e.tensor_reduce(out=ssum, in_=gw, axis=AX.X, op=ALU.add))
    V_(lambda e: e.reciprocal(out=rs, in_=ssum))
    V_(lambda e: e.tensor_scalar(out=Gout, in0=gw, scalar1=rs, scalar2=ROUTED_SCALE, op0=ALU.mult, op1=ALU.mult))
    return va


def _alibi_bias():
    slopes = (2.0 ** (-8.0 * np.arange(1, 17, dtype=np.float64) / 16)).astype(np.float32)
    k = np.arange(128)[:, None].astype(np.float32)
    q = np.arange(128)[None, :].astype(np.float32)
    out = np.zeros((128, 4, 2, 2, 2, 128), np.float32)
    for g in range(4):
        for par in range(2):
            for hh in range(2):
                h = 4 * g + 2 * hh + par
                dist_prev = q + 128 - k
                dist_cur = q - k
                out[:, g, par, 0, hh, :] = np.where(dist_prev < 128, -slopes[h] * dist_prev, NEG)
                out[:, g, par, 1, hh, :] = np.where(dist_cur >= 0, -slopes[h] * dist_cur, NEG)
    return out.reshape(128, 4, 2, 512)


_CACHE = {}


def _prep_inputs(inputs):
    f = lambda a: np.ascontiguousarray(np.asarray(a, dtype=np.float32))
    x = f(inputs["x"])
    c = f(inputs["c"])
    shared = {
        "w_mod": f(inputs["w_mod"][0]),
        "b_mod": f(inputs["b_mod"]).reshape(1, -1),
        "bmod_col": f(np.asarray(inputs["b_mod"]).reshape(96, 128).T),
        "w_in": f(inputs["w_in"][0]),
        "convw": f(np.asarray(inputs["conv_w"][0]).reshape(3, 8, 128).transpose(2, 1, 0)),
        "sinks": f(np.asarray(inputs["attn_sinks"][0]).reshape(8, 2)[:, :, None].repeat(64, axis=2).reshape(8, 128).T),
        "w_out": f(inputs["w_out"][0]),
        "ln1_g": f(inputs["ln1_g"]).reshape(1, -1),
        "ln1_b": f(inputs["ln1_b"]).reshape(1, -1),
        "ln2_g": f(inputs["ln2_g"]).reshape(1, -1),
        "ln2_b": f(inputs["ln2_b"]).reshape(1, -1),
        "w_router": f(inputs["w_router"][0]),
        "router_bias": f(inputs["router_bias"]).reshape(1, -1),
        "w_gate": f(inputs["w_gate"][0]),
        "w_up": f(inputs["w_up"][0]),
        "w_down": f(inputs["w_down"][0]),
        "ws_gate": f(inputs["ws_gate"][0]),
        "ws_up": f(inputs["ws_up"][0]),
        "ws_down": f(inputs["ws_down"][0]),
        "abias": _alibi_bias(),
        "ident_in": np.eye(128, dtype=np.float32),
        "ltri": np.triu(np.ones((128, 128), np.float32), k=1),
        "ebase": np.broadcast_to((np.arange(NE, dtype=np.float32) * CAP)[None, :], (128, NE)).copy(),
        "pcol": (np.arange(128, dtype=np.float32)[:, None] + 128.0 * np.arange(NS, dtype=np.float32)[None, :]).copy(),
    }
    in_maps = []
    for i in range(NCORES):
        b, ch = divmod(i, 4)
        t0 = ch * TOWN
        xh = np.zeros((TALL, D), np.float32)
        xh[128:] = x[b, t0:t0 + TOWN]
        flags = np.zeros((128, 2), np.float32)
        if ch > 0:
            xh[:128] = x[b, t0 - 128:t0]
            flags[:, 1] = 1.0
        else:
            flags[:, 0] = NEG
        m = dict(shared)
        m["xh"] = xh
        m["ccol"] = np.ascontiguousarray(c[b].reshape(KC, 128).T)
        m["flags"] = flags
        in_maps.append(m)
    return in_maps


def kernel(**inputs):
    if "nc" not in _CACHE:
        _CACHE["nc"] = build_program()
    nc = _CACHE["nc"]
    in_maps = _prep_inputs(inputs)
    res = run_bass_kernel_spmd(nc, in_maps, core_ids=list(range(NCORES)))
    out = np.empty((2, 4096, D), np.float32)
    for i in range(NCORES):
        b, ch = divmod(i, 4)
        out[b, ch * TOWN:(ch + 1) * TOWN] = res.results[i]["y"]
    return out
```
